# Optimizing a Trainium2 kernel written in Bass

```python
import math
import jax, jax.numpy as jnp
from jax import lax
import numpy as np

D_MODEL = 1024
BATCH = 16
SEQ = 2048
DEPTH = 2

CTX_LEN = 256
GRID_W = 64
MIX_WIDTH = D_MODEL
ATTN_HEADS = 4
ATTN_QK_DIM = 64
ATTN_V_DIM = 2 * ATTN_QK_DIM
Q_W = ATTN_HEADS * 2 * ATTN_QK_DIM
ATTN_WIDTH = ATTN_HEADS * ATTN_V_DIM
CONV_WIDTH = MIX_WIDTH - ATTN_WIDTH
CONV_GROUPS = 8
CONV_K = 3
K_OFF = Q_W
V_OFF = 2 * Q_W
B_OFF = V_OFF + ATTN_WIDTH
C_OFF = B_OFF + CONV_WIDTH
X_OFF = C_OFF + CONV_WIDTH
EVEN_IN = X_OFF + CONV_WIDTH
CHUNK = 128
CMLP_GROUPS = 4
CMLP_WIDTH = MIX_WIDTH
N_EXPERTS = 16
EXPERT_HIDDEN = D_MODEL
CAPACITY_FACTOR = 2
ROPE_THETA = 10000.0
NORM_EPS = 1e-6
Q_BLOCK = 128
N_EVEN = (DEPTH + 1) // 2
N_ODD = DEPTH // 2

kernel_name = "hybrid_diffattn_shortconv_chunkmlp_ecmoe_dit"


def _rms(x, g):
    xf = x.astype(jnp.float32)
    y = xf * lax.rsqrt(jnp.mean(xf * xf, axis=-1, keepdims=True) + NORM_EPS)
    return y.astype(x.dtype) * g


def _modulate(h, shift, scale):
    return h * (1 + scale) + shift


def _ctx_needed_after(l):
    return any(j % 2 == 0 for j in range(l + 1, DEPTH))


def _axial_rope_tables(n):
    rows = n // GRID_W
    row = jnp.repeat(jnp.arange(rows), GRID_W).astype(jnp.float32)
    col = jnp.tile(jnp.arange(GRID_W), rows).astype(jnp.float32)
    half = ATTN_QK_DIM // 2
    inv = 1.0 / (ROPE_THETA ** (jnp.arange(0, half, 2, dtype=jnp.float32) / half))
    ang_r = row[:, None] * inv
    ang_c = col[:, None] * inv
    ang = jnp.concatenate([ang_r, ang_r, ang_c, ang_c], axis=-1)
    return jnp.cos(ang), jnp.sin(ang)


def _apply_rope(x, cos, sin):
    quarter = ATTN_QK_DIM // 4
    xs = x.reshape(x.shape[:-1] + (2, 2, quarter))
    rot = jnp.stack([-xs[..., 1, :], xs[..., 0, :]], axis=-2).reshape(x.shape)
    cb = cos[None, :, None, None, :]
    sb = sin[None, :, None, None, :]
    return (x.astype(jnp.float32) * cb + rot.astype(jnp.float32) * sb).astype(x.dtype)


def _diff_attention(q, k, v, lam):
    bsz, n, heads, _, d = q.shape
    nb = n // Q_BLOCK
    qb = q.reshape(bsz, nb, Q_BLOCK, heads, 2, d).transpose(1, 0, 3, 4, 2, 5)
    kt = k.transpose(0, 2, 3, 1, 4)
    vt = v.transpose(0, 2, 1, 3)
    scale = d ** -0.5

    def one_block(q_blk):
        s = jnp.einsum('bhmqd,bhmkd->bhmqk', q_blk, kt).astype(jnp.float32) * scale
        p = jax.nn.softmax(s, axis=-1)
        a = p[:, :, 0] - lam * p[:, :, 1]
        return jnp.einsum('bhqk,bhkv->bhqv', a.astype(vt.dtype), vt)

    o = lax.map(one_block, qb)
    return o.transpose(1, 0, 3, 2, 4).reshape(bsz, n, heads, -1)


def _short_conv(u, w):
    n = u.shape[1]
    pad = CONV_K // 2
    up = jnp.pad(u, ((0, 0), (pad, pad), (0, 0)))
    return sum(up[:, i:i + n] * w[i] for i in range(CONV_K))


def _diff_heads(qp, kp, vp, lam, subln, lam_init, cos=None, sin=None, k_prefix=None, v_prefix=None):
    bsz, n, _ = qp.shape
    q = qp.reshape(bsz, n, ATTN_HEADS, 2, ATTN_QK_DIM)
    k = kp.reshape(bsz, kp.shape[1], ATTN_HEADS, 2, ATTN_QK_DIM)
    v = vp.reshape(bsz, vp.shape[1], ATTN_HEADS, ATTN_V_DIM)
    if cos is not None:
        q = _apply_rope(q, cos, sin)
        k = _apply_rope(k, cos, sin)
    if k_prefix is not None:
        k = jnp.concatenate([k_prefix, k], axis=1)
        v = jnp.concatenate([v_prefix, v], axis=1)
    o = _diff_attention(q, k, v, lam)
    o = _rms(o, subln) * (1.0 - lam_init)
    return o.reshape(bsz, n, ATTN_WIDTH)


def _even_mixer(h, hc, w_in, lam_p, subln, conv_w, cos, sin, lam_init, ctx_out):
    bsz, m = hc.shape[0], hc.shape[1]
    lam = (jnp.exp(jnp.sum(lam_p[0] * lam_p[1]).astype(jnp.float32))
           - jnp.exp(jnp.sum(lam_p[2] * lam_p[3]).astype(jnp.float32)) + lam_init)
    p = h @ w_in
    q, k, v, gb, gc, xs = jnp.split(p, [K_OFF, V_OFF, B_OFF, C_OFF, X_OFF], axis=-1)
    if ctx_out:
        pc = hc @ w_in
        qc, kc, vc, gbc, gcc, xsc = jnp.split(pc, [K_OFF, V_OFF, B_OFF, C_OFF, X_OFF], axis=-1)
    else:
        kvc = hc @ w_in[:, K_OFF:B_OFF]
        kc, vc = jnp.split(kvc, [V_OFF - K_OFF], axis=-1)
    kc_h = kc.reshape(bsz, m, ATTN_HEADS, 2, ATTN_QK_DIM)
    vc_h = vc.reshape(bsz, m, ATTN_HEADS, ATTN_V_DIM)
    attn = _diff_heads(q, k, v, lam, subln, lam_init, cos, sin, kc_h, vc_h)
    conv = gb * _short_conv(gc * xs, conv_w)
    mix = jnp.concatenate([attn, conv], axis=-1)
    mix_c = None
    if ctx_out:
        attn_c = _diff_heads(qc, kc, vc, lam, subln, lam_init)
        conv_c = gbc * _short_conv(gcc * xsc, conv_w)
        mix_c = jnp.concatenate([attn_c, conv_c], axis=-1)
    return mix, mix_c


def _chunk_mlp(h, w_in, v_norm, w_s, b_s):
    bsz, n, _ = h.shape
    p = jax.nn.gelu(h @ w_in, approximate=False)
    u, v = jnp.split(p, 2, axis=-1)
    v = _rms(v, v_norm)
    gw = CMLP_WIDTH // CMLP_GROUPS
    vc = v.reshape(bsz, n // CHUNK, CHUNK, CMLP_GROUPS, gw)
    s = jnp.einsum('gij,bcjgw->bcigw', w_s, vc) + b_s.T[None, None, :, :, None]
    return u * s.reshape(bsz, n, CMLP_WIDTH)


def _expert_choice_moe(h, w_r, w_gate, w_up, w_down):
    bsz, n, d = h.shape
    cap = CAPACITY_FACTOR * n // N_EXPERTS
    probs = jax.nn.softmax((h @ w_r).astype(jnp.float32), axis=-1)
    vals, idx = lax.top_k(jnp.swapaxes(probs, 1, 2), cap)
    xs = jax.vmap(lambda hb, ib: hb[ib])(h, idx)
    a = jnp.einsum('becd,edf->becf', xs, w_gate)
    b = jnp.einsum('becd,edf->becf', xs, w_up)
    y = jnp.einsum('becf,efd->becd', jax.nn.silu(a) * b, w_down) * vals[..., None].astype(h.dtype)
    return jax.vmap(lambda yb, ib: jnp.zeros((n, d), yb.dtype).at[ib.reshape(-1)].add(yb.reshape(-1, d)))(y, idx)


def setup_inputs(seed: int = 0) -> dict:
    key = jax.random.key(seed)
    ks = jax.random.split(key, 24)
    nrm = lambda k, shape, s: jax.random.normal(k, shape, jnp.float32) * s
    D = D_MODEL
    return {
        "x": nrm(ks[0], (BATCH, SEQ, D), 1.0),
        "c": nrm(ks[1], (BATCH, D), 1.0),
        "ctx": nrm(ks[2], (BATCH, CTX_LEN, D), 1.0),
        "c_ctx": nrm(ks[3], (D,), 1.0),
        "w_mod": nrm(ks[4], (DEPTH, D, 6 * D), 0.5 * D ** -0.5),
        "b_mod": nrm(ks[5], (DEPTH, 6 * D), 0.02),
        "norm1": 1.0 + nrm(ks[6], (DEPTH, D), 0.02),
        "norm2": 1.0 + nrm(ks[7], (DEPTH, D), 0.02),
        "even_w_in": nrm(ks[8], (N_EVEN, D, EVEN_IN), D ** -0.5),
        "even_lambda": nrm(ks[9], (N_EVEN, 4, ATTN_QK_DIM), 0.1),
        "even_subln": 1.0 + nrm(ks[10], (N_EVEN, ATTN_V_DIM), 0.02),
        "even_conv_w": nrm(ks[11], (N_EVEN, CONV_K, CONV_WIDTH), CONV_K ** -0.5),
        "odd_w_in": nrm(ks[12], (N_ODD, D, 2 * CMLP_WIDTH), D ** -0.5),
        "odd_v_norm": 1.0 + nrm(ks[13], (N_ODD, CMLP_WIDTH), 0.02),
        "odd_w_s": nrm(ks[14], (N_ODD, CMLP_GROUPS, CHUNK, CHUNK), CHUNK ** -0.5),
        "odd_b_s": 1.0 + nrm(ks[15], (N_ODD, CMLP_GROUPS, CHUNK), 0.01),
        "w_out": nrm(ks[16], (DEPTH, MIX_WIDTH, D), MIX_WIDTH ** -0.5),
        "w_router": nrm(ks[17], (DEPTH, D, N_EXPERTS), D ** -0.5),
        "w_gate": nrm(ks[18], (DEPTH, N_EXPERTS, D, EXPERT_HIDDEN), D ** -0.5),
        "w_up": nrm(ks[19], (DEPTH, N_EXPERTS, D, EXPERT_HIDDEN), D ** -0.5),
        "w_down": nrm(ks[20], (DEPTH, N_EXPERTS, EXPERT_HIDDEN, D), EXPERT_HIDDEN ** -0.5),
        "final_norm": 1.0 + nrm(ks[21], (D,), 0.02),
    }


def reference(x, c, ctx, c_ctx, w_mod, b_mod, norm1, norm2, even_w_in, even_lambda, even_subln,
              even_conv_w, odd_w_in, odd_v_norm, odd_w_s, odd_b_s, w_out, w_router, w_gate, w_up,
              w_down, final_norm):
    n_lat = x.shape[1]
    cos, sin = _axial_rope_tables(n_lat)
    xc = ctx
    sc_c = jax.nn.silu(c)
    sc_ctx = jax.nn.silu(c_ctx)
    for l in range(DEPTH):
        ctx_out = _ctx_needed_after(l)
        need_c = (l % 2 == 0) or ctx_out
        mod = sc_c @ w_mod[l] + b_mod[l]
        sh1, scl1, g1, sh2, scl2, g2 = jnp.split(mod[:, None, :], 6, axis=-1)
        h = _modulate(_rms(x, norm1[l]), sh1, scl1)
        if need_c:
            modc = sc_ctx @ w_mod[l] + b_mod[l]
            csh1, cscl1, cg1, csh2, cscl2, cg2 = jnp.split(modc, 6)
            hc = _modulate(_rms(xc, norm1[l]), csh1, cscl1)
        if l % 2 == 0:
            e = l // 2
            lam_init = 0.8 - 0.6 * math.exp(-0.3 * l)
            mix, mix_c = _even_mixer(h, hc, even_w_in[e], even_lambda[e], even_subln[e],
                                     even_conv_w[e], cos, sin, lam_init, ctx_out)
        else:
            o = l // 2
            mix = _chunk_mlp(h, odd_w_in[o], odd_v_norm[o], odd_w_s[o], odd_b_s[o])
            mix_c = _chunk_mlp(hc, odd_w_in[o], odd_v_norm[o], odd_w_s[o], odd_b_s[o]) if ctx_out else None
        x = x + g1 * (mix @ w_out[l])
        h2 = _modulate(_rms(x, norm2[l]), sh2, scl2)
        x = x + g2 * _expert_choice_moe(h2, w_router[l], w_gate[l], w_up[l], w_down[l])
        if ctx_out:
            xc = xc + cg1 * (mix_c @ w_out[l])
            hc2 = _modulate(_rms(xc, norm2[l]), csh2, cscl2)
            xc = xc + cg2 * _expert_choice_moe(hc2, w_router[l], w_gate[l], w_up[l], w_down[l])
    return _rms(x, final_norm)
```

```python
import contextlib
import math
import numpy as np
import concourse.bass as bass
import concourse.mybir as mybir
from concourse.bass_utils import run_bass_kernel_spmd

F32 = mybir.dt.float32
BF16 = mybir.dt.bfloat16
U32 = mybir.dt.uint32
I32 = mybir.dt.int32
ALU = mybir.AluOpType
AF = mybir.ActivationFunctionType
AX = mybir.AxisListType

N_CORES = 8
D = 1024
SEQ = 2048
CTX = 256
NS = 2
NE = 16
CAP = 256
EPS = 1e-6


class Res:
    __slots__ = ("name", "w", "r", "dsem")

    def __init__(self, name):
        self.name = name
        self.w = None
        self.r = {}
        self.dsem = None


class K:
    def __init__(self, nc, n_dma_sems=96, same_engine_sync=True):
        self.nc = nc
        self.stack = contextlib.ExitStack()
        self.eng = {"pe": nc.tensor, "act": nc.scalar, "dve": nc.vector, "pool": nc.gpsimd, "sp": nc.sync}
        self.sems = {}
        self.cnt = {}
        for e in self.eng:
            self.sems[e] = self.stack.enter_context(nc.semaphore("sem_" + e))
            self.cnt[e] = 0
        self.dsems = {"hw": [], "sw": []}
        self.dsem_i = {"hw": 0, "sw": 0}
        for i in range(n_dma_sems):
            k = "d%d" % i
            self.sems[k] = self.stack.enter_context(nc.semaphore("sem_" + k))
            self.cnt[k] = 0
            self.dsems["sw" if i % 3 == 2 else "hw"].append(k)
        self.waited = {e: {} for e in self.eng}
        self.same_engine_sync = same_engine_sync
        self.n_inst = 0
        self.n_wait = 0

    def _deps(self, reads, writes, eng=None):
        deps = {}
        def add(ev):
            if ev is None:
                return
            k, v = ev
            if deps.get(k, 0) < v:
                deps[k] = v
        for r in reads:
            add(r.w)
        for w in writes:
            if not (eng is not None and w.w is not None and w.w[0] == eng):
                add(w.w)
            for k, v in w.r.items():
                add((k, v))
        return deps

    def _wait(self, e, deps):
        wd = self.waited[e]
        for k, v in deps.items():
            if k == e and (e == "pe" or not self.same_engine_sync):
                continue
            if wd.get(k, 0) >= v:
                continue
            self.eng[e].wait_ge(self.sems[k], v)
            self.n_wait += 1
            wd[k] = v

    def _mark(self, ev, reads, writes):
        k, v = ev
        for r in reads:
            if r.r.get(k, 0) < v:
                r.r[k] = v
        for w in writes:
            w.w = ev
            w.r = {}

    def op(self, e, fn, reads=(), writes=()):
        self._wait(e, self._deps(reads, writes, eng=e))
        inst = fn(self.eng[e])
        self.cnt[e] += 1
        inst.then_inc(self.sems[e], 1)
        self._mark((e, self.cnt[e]), reads, writes)
        self.n_inst += 1
        return inst

    def _dma_ev(self, q, inst_fn):
        kind = "sw" if q == "pool" else "hw"
        pool = self.dsems[kind]
        k = pool[self.dsem_i[kind] % len(pool)]
        self.dsem_i[kind] += 1
        if self.cnt[k]:
            self._wait(q, {k: self.cnt[k]})
        inst = inst_fn()
        self.cnt[k] += 16
        inst.then_inc(self.sems[k], 16)
        self.n_inst += 1
        return (k, self.cnt[k])

    def dma(self, q, out, in_, reads, writes, sres=None, **kw):
        self._wait(q, self._deps(reads, writes))
        ev = self._dma_ev(q, lambda: self.eng[q].dma_start(out=out, in_=in_, **kw))
        self._mark(ev, reads, writes)

    def idma(self, out, out_offset, in_, in_offset, reads, writes, sres=None, **kw):
        self._wait("pool", self._deps(reads, writes))
        ev = self._dma_ev("pool", lambda: self.nc.gpsimd.indirect_dma_start(
            out=out, out_offset=out_offset, in_=in_, in_offset=in_offset, **kw))
        self._mark(ev, reads, writes)

    def barrier(self, release=True):
        evs = {}
        for e in self.eng:
            if self.cnt[e]:
                evs[e] = self.cnt[e]
        for k in self.sems:
            if k not in self.eng and self.cnt[k]:
                evs[k] = self.cnt[k]
        for e in self.eng:
            d = dict(evs)
            d.pop(e, None) if e == "pe" else None
            self._wait(e, d)

    def finish(self):
        self.barrier(release=False)
        self.stack.close()


class T:
    def __init__(self, h, name):
        self.h = h
        self.r = Res(name)

    def __getitem__(self, idx):
        return self.h[idx]


def build_nc(stop="all"):
    nc = bass.Bass("TRN2", target_bir_lowering=False)

    def din(name, shape, dt=F32):
        return nc.dram_tensor(name, list(shape), dt, kind="ExternalInput").ap()

    def dscr(name, shape, dt):
        return nc.dram_tensor(name, list(shape), dt, kind="Internal").ap()

    x_in = din("x", [NS, SEQ, D])
    cvec = din("cvec", [4, D])
    ctx_in = din("ctx", [NS, CTX, D])
    w_mod = din("w_mod", [2, D, 6 * D])
    b_mod = din("b_mod", [2, 6 * D])
    norm1 = din("norm1", [2, D])
    norm2 = din("norm2", [2, D])
    even_w_in = din("even_w_in", [D, 3072])
    even_lambda = din("even_lambda", [4, 64])
    even_subln = din("even_subln", [128])
    even_conv_w = din("even_conv_w", [3, 512])
    odd_w_in = din("odd_w_in", [D, 2048])
    odd_v_norm = din("odd_v_norm", [D])
    odd_w_s = din("odd_w_s", [4, 128, 128])
    odd_b_s = din("odd_b_s", [4, 128])
    w_out = din("w_out", [2, D, D])
    w_router = din("w_router", [2, D, NE])
    w_gate = din("w_gate", [2, NE, D, D])
    w_up = din("w_up", [2, NE, D, D])
    w_down = din("w_down", [2, NE, D, D])
    final_norm = din("final_norm", [D])
    rope_cos = din("rope_cos", [128, SEQ])
    rope_sin = din("rope_sin", [128, SEQ])
    out_d = nc.dram_tensor("out", [NS, SEQ, D], F32, kind="ExternalOutput").ap()
    if stop != "all":
        dbg_idx = nc.dram_tensor("dbg_idx", [128, 2, 64], I32, kind="ExternalOutput").ap()
        dbg_val = nc.dram_tensor("dbg_val", [128, 2, 64], F32, kind="ExternalOutput").ap()

    xd = [dscr("xd%d" % s_, [SEQ, D], F32) for s_ in range(NS)]
    h2d = [dscr("h2d%d" % s_, [SEQ, D], BF16) for s_ in range(NS)]
    qTd = dscr("qTd", [NS, 4, 128, SEQ], BF16)
    kTd = dscr("kTd", [NS, 4, 128, SEQ + CTX], BF16)
    vd = dscr("vd", [NS, SEQ + CTX, 512], BF16)
    ud = dscr("ud", [NS, 512, SEQ], F32)
    gbd = dscr("gbd", [NS, 512, SEQ], F32)
    mixTd = dscr("mixTd", [NS, D, SEQ], BF16)

    k = K(nc)
    dummy = Res("dummy")
    NKV = SEQ + CTX
    with contextlib.ExitStack() as gst, nc.allow_non_contiguous_dma(reason="small strided parameter loads"):
        cnt = [0]

        def alloc(st, shape, dt, name=None):
            cnt[0] += 1
            nm = "%s_%d" % (name or "t", cnt[0])
            return T(st.enter_context(nc.sbuf_tensor(nm, list(shape), dt)), nm)

        G = lambda shape, dt, name=None: alloc(gst, shape, dt, name)
        class V:
            def __init__(self, ap, name):
                self.ap = ap
                self.r = Res(name)

            def __getitem__(self, idx):
                return self.ap[idx]

        psd = [gst.enter_context(nc.psum_tensor("psd%d" % i, [128, 1024], F32)) for i in range(4)]
        ps = [V(psd[i // 2][:, (i % 2) * 512:(i % 2 + 1) * 512], "psb%d" % i) for i in range(8)]
        psi = [0]

        def nps():
            psi[0] += 1
            return ps[psi[0] % 8]

        def bank_pool(banks):
            c = [0]

            def f():
                c[0] += 1
                return ps[banks[c[0] % len(banks)]]
            return f

        def run_skewed(n, stages, extra=None):
            S = len(stages)
            for t in range(n + S - 1):
                if extra is not None:
                    extra(t)
                for j in reversed(range(S)):
                    i = t - j
                    if 0 <= i < n:
                        stages[j](i)

        identf = G([128, 128], F32, "identf")
        identb = G([128, 128], BF16, "identb")
        onesf = G([128, 128], F32, "onesf")
        k.op("pool", lambda e: e.memset(identf[:], 0.0), writes=[identf.r])
        k.op("pool", lambda e: e.affine_select(out=identf[:], in_=identf[:], pattern=[[-1, 128]],
                                               compare_op=ALU.not_equal, fill=1.0, base=0, channel_multiplier=1),
             reads=[identf.r], writes=[identf.r])
        k.op("dve", lambda e: e.tensor_copy(out=identb[:], in_=identf[:]), reads=[identf.r], writes=[identb.r])
        k.op("pool", lambda e: e.memset(onesf[:], 1.0), writes=[onesf.r])
        epsb = G([128, 1], F32, "epsb")
        k.op("pool", lambda e: e.memset(epsb[:], EPS), writes=[epsb.r])

        n1T = G([128, 2, 8], F32, "n1T")
        n2T = G([128, 2, 8], F32, "n2T")
        bmT = G([128, 2, 48], F32, "bmT")
        fnT = G([128, 8], F32, "fnT")
        vnT = G([128, 8], F32, "vnT")
        slT = G([128, 1], F32, "slT")
        cwT = G([128, 3, 4], F32, "cwT")
        bsT = G([128, 4], F32, "bsT")
        scT = G([128, 8, 4], F32, "scT")
        wr = [G([128, 8, NE], F32, "wr%d" % l) for l in range(2)]
        lamb = G([128, 4, 64], F32, "lamb")
        k.dma("sp", n1T[:], norm1.rearrange("l (j p) -> p l j", p=128), [], [n1T.r])
        k.dma("sp", n2T[:], norm2.rearrange("l (j p) -> p l j", p=128), [], [n2T.r])
        k.dma("sp", bmT[:], b_mod.rearrange("l (j p) -> p l j", p=128), [], [bmT.r])
        k.dma("sp", fnT[:], final_norm.rearrange("(j p) -> p j", p=128), [], [fnT.r])
        k.dma("sp", vnT[:], odd_v_norm.rearrange("(j p) -> p j", p=128), [], [vnT.r])
        k.dma("sp", slT[:], even_subln.rearrange("(j p) -> p j", p=128), [], [slT.r])
        k.dma("sp", cwT[:], even_conv_w.rearrange("i (c p) -> p i c", p=128), [], [cwT.r])
        k.dma("sp", bsT[:], odd_b_s.rearrange("g i -> i g"), [], [bsT.r])
        for r in range(4):
            k.dma("sp", scT[:, :, r], cvec[r].rearrange("(j p) -> p j", p=128), [scT.r], [scT.r])
        for l in range(2):
            k.dma("sp", wr[l][:], w_router[l].rearrange("(j p) e -> p j e", p=128), [], [wr[l].r])
        k.dma("sp", lamb[:], even_lambda.partition_broadcast(128), [], [lamb.r])
        k.op("act", lambda e: e.activation(out=scT[:], in_=scT[:], func=AF.Silu), reads=[scT.r], writes=[scT.r])

        LAM_INIT = 0.8 - 0.6 * math.exp(-0.3 * 0)
        lprod = G([128, 2, 64], F32, "lprod")
        lsum = G([128, 2], F32, "lsum")
        lamn = G([128, 1], F32, "lamn")
        k.op("dve", lambda e: e.tensor_tensor(out=lprod[:, 0, :], in0=lamb[:, 0, :], in1=lamb[:, 1, :], op=ALU.mult),
             reads=[lamb.r], writes=[lprod.r])
        k.op("dve", lambda e: e.tensor_tensor(out=lprod[:, 1, :], in0=lamb[:, 2, :], in1=lamb[:, 3, :], op=ALU.mult),
             reads=[lamb.r, lprod.r], writes=[lprod.r])
        k.op("dve", lambda e: e.tensor_reduce(out=lsum[:], in_=lprod[:], axis=AX.X, op=ALU.add),
             reads=[lprod.r], writes=[lsum.r])
        k.op("act", lambda e: e.activation(out=lsum[:], in_=lsum[:], func=AF.Exp), reads=[lsum.r], writes=[lsum.r])
        k.op("dve", lambda e: e.tensor_tensor(out=lamn[:], in0=lsum[:, 1:2], in1=lsum[:, 0:1], op=ALU.subtract),
             reads=[lsum.r], writes=[lamn.r])
        k.op("dve", lambda e: e.tensor_scalar(out=lamn[:], in0=lamn[:], scalar1=-LAM_INIT, scalar2=None, op0=ALU.add),
             reads=[lamn.r], writes=[lamn.r])

        modT = [G([128, 48, 4], F32, "modT%d" % l) for l in range(2)]
        a1T = [G([128, 8, 4], F32, "a1T%d" % l) for l in range(2)]
        a2T = [G([128, 8, 4], F32, "a2T%d" % l) for l in range(2)]
        def mod_items(l, st, nbuf, bank_fn):
            wmb = [alloc(st, [128, 8, 512], F32, "wmb") for _ in range(nbuf)]
            modrow = alloc(st, [4, 6 * D], F32, "modrow")
            brow = alloc(st, [4, 6 * D], F32, "brow")
            items = []

            def blk(jb):
                if jb == 0:
                    k.dma("sp", brow[:], b_mod[l].partition_broadcast(4), [], [brow.r])
                wm = wmb[jb % nbuf]
                k.dma("sp", wm[:], w_mod[l][:, jb * 512:(jb + 1) * 512].rearrange("(j p) n -> p j n", p=128),
                      [], [wm.r])
                pb = bank_fn()

                def mm(e):
                    for kc in range(8):
                        i = e.matmul(pb[0:4, :], lhsT=scT[:, kc, :], rhs=wm[:, kc, :], start=(kc == 0), stop=(kc == 7))
                    return i
                k.op("pe", mm, reads=[wm.r, scT.r], writes=[pb.r])
                k.op("dve", lambda e: e.tensor_tensor(
                    out=modrow[:, jb * 512:(jb + 1) * 512], in0=pb[0:4, :], in1=brow[:, jb * 512:(jb + 1) * 512],
                    op=ALU.add), reads=[pb.r, brow.r], writes=[modrow.r])

            def fin():
                pm = bank_fn()

                def trs(e):
                    for j in range(48):
                        i = e.transpose(out=pm[:, j * 4:(j + 1) * 4], in_=modrow[0:4, j * 128:(j + 1) * 128],
                                        identity=identf[0:4, 0:4])
                    return i
                k.op("pe", trs, reads=[modrow.r, identf.r], writes=[pm.r])
                k.op("dve", lambda e: e.tensor_copy(out=modT[l][:].rearrange("p j r -> p (j r)"), in_=pm[:, 0:192]),
                     reads=[pm.r], writes=[modT[l].r])
                for r in range(3):
                    k.op("dve", lambda e, r=r: e.scalar_tensor_tensor(
                        out=a1T[l][:, :, r], in0=modT[l][:, 8:16, r], scalar=1.0, in1=n1T[:, l, :],
                        op0=ALU.add, op1=ALU.mult), reads=[modT[l].r, n1T.r, a1T[l].r], writes=[a1T[l].r])
                    k.op("dve", lambda e, r=r: e.scalar_tensor_tensor(
                        out=a2T[l][:, :, r], in0=modT[l][:, 32:40, r], scalar=1.0, in1=n2T[:, l, :],
                        op0=ALU.add, op1=ALU.mult), reads=[modT[l].r, n2T.r, a2T[l].r], writes=[a2T[l].r])
            for jb in range(12):
                items.append(lambda jb=jb: blk(jb))
            items.append(fin)
            return items

        with contextlib.ExitStack() as st:
            for it in mod_items(0, st, 3, nps):
                it()
            k.barrier()

        diag = [G([128, 128], F32, "diag") for _ in range(2)]
        dgi = [0]

        def bcast(dst, col_fn, n, srcres):
            for q in range((n + 3) // 4):
                pb = nps()
                for jj in range(min(4, n - q * 4)):
                    j = q * 4 + jj
                    dg = diag[dgi[0] % 2]
                    dgi[0] += 1
                    k.op("dve", lambda e, dg=dg, j=j: e.tensor_scalar(out=dg[:], in0=identf[:], scalar1=col_fn(j),
                                                                     scalar2=None, op0=ALU.mult),
                         reads=[identf.r, srcres], writes=[dg.r])
                    k.op("pe", lambda e, dg=dg, jj=jj, pb=pb: e.matmul(pb[:, jj * 128:(jj + 1) * 128], lhsT=onesf[:],
                                                                        rhs=dg[:], start=True, stop=True),
                         reads=[onesf.r, dg.r], writes=[pb.r])
                w = min(4, n - q * 4) * 128
                k.op("act", lambda e, pb=pb, q=q, w=w: e.copy(out=dst[:, q * 512:q * 512 + w], in_=pb[:, 0:w]),
                     reads=[pb.r], writes=[dst.r])

        def rstd_from_ss(ss, rs, n, scale):
            k.op("act", lambda e: e.activation(out=rs[:, 0:n], in_=ss[:, 0:n], func=AF.Ln, bias=epsb[:, 0:1], scale=scale),
                 reads=[ss.r, epsb.r], writes=[rs.r])
            k.op("act", lambda e: e.activation(out=rs[:, 0:n], in_=rs[:, 0:n], func=AF.Exp, scale=-0.5),
                 reads=[rs.r], writes=[rs.r])

        probsT = G([64, SEQ], F32, "probsT")
        tvals = G([64, CAP], F32, "tvals")
        tidx = G([64, CAP], U32, "tidx")
        tidxf = G([64, CAP], F32, "tidxf")
        idxT = G([128, 2, 64], I32, "idxT")
        valT = G([128, 2, 64], F32, "valT")
        k.op("pool", lambda e: e.memset(probsT[:], 0.0), writes=[probsT.r])

        def phase_l0_inproj():
            with contextlib.ExitStack() as st:
                A = lambda shape, dt, name=None: alloc(st, shape, dt, name)
                win = A([128, 8, 3072], BF16, "win")
                wrot = A([128, 8, 1024], BF16, "wrot")
                cosT = A([128, SEQ], F32, "cosT")
                sinT = A([128, SEQ], F32, "sinT")
                for cb in range(3):
                    k.dma("pool", win[:, :, cb * 1024:(cb + 1) * 1024],
                          even_w_in[:, cb * 1024:(cb + 1) * 1024].rearrange("(j p) n -> p j n", p=128), [], [win.r])
                k.dma("sp", cosT[:], rope_cos, [], [cosT.r])
                k.dma("sp", sinT[:], rope_sin, [], [sinT.r])
                for kc in range(8):
                    src = win[:, kc, 0:1024].rearrange("p (g h c) -> p g h c", h=2, c=16)
                    dst = wrot[:, kc, :].rearrange("p (g h c) -> p g h c", h=2, c=16)
                    k.op("act", lambda e, src=src, dst=dst: e.mul(dst[:, :, 0, :], src[:, :, 1, :], -1.0),
                         reads=[win.r], writes=[wrot.r])
                    k.op("dve", lambda e, src=src, dst=dst: e.tensor_copy(out=dst[:, :, 1, :], in_=src[:, :, 0, :]),
                         reads=[win.r], writes=[wrot.r])
                xts = [A([128, 4, D], F32, "xt") for _ in range(2)]
                hTs = [A([128, 8, 512], BF16, "hT") for _ in range(2)]
                junk = A([128, D], BF16, "junk")
                ss = A([128, 4], F32, "ss")
                rs = A([128, 4], F32, "rs")
                qst = A([128, 4, 512], BF16, "qst")
                kst = A([128, 4, 512], BF16, "kst")
                ust = A([128, 4, 512], F32, "ust")
                gbst = A([128, 4, 512], F32, "gbst")
                vst = A([128, 4, 512], BF16, "vst")
                gcsb = [A([128, 512], F32, "gcsb") for _ in range(2)]
                t1s = [A([128, 512], F32, "t1") for _ in range(2)]
                t2s = [A([128, 512], F32, "t2") for _ in range(2)]
                work = []
                for s in range(NS):
                    work.append((s, "ctx", 0))
                    for tb in range(4):
                        work.append((s, "lat", tb))

                def load(i):
                    s, kind, tb = work[i]
                    xt = xts[i % 2]
                    if kind == "ctx":
                        k.dma("sp", xt[:, 0:2, :], ctx_in[s].rearrange("(c p) d -> p c d", p=128), [], [xt.r])
                    else:
                        k.dma("sp", xt[:], x_in[s][tb * 512:(tb + 1) * 512, :].rearrange("(c p) d -> p c d", p=128),
                              [], [xt.r])

                ri = [0]

                def meta(i):
                    s, kind, tb = work[i]
                    nch = 2 if kind == "ctx" else 4
                    return s, kind, tb, nch, nch * 128, (2 if kind == "ctx" else s)

                pA = bank_pool([0, 1])
                pB = bank_pool([2, 3, 4])
                pC = bank_pool([5, 6, 7])

                def s_norm(i):
                    s, kind, tb, nch, ntok, r = meta(i)
                    xt, hT = xts[i % 2], hTs[i % 2]
                    for c in range(nch):
                        k.op("act", lambda e, c=c: e.activation(out=junk[:], in_=xt[:, c, :], func=AF.Square,
                                                                accum_out=ss[:, c:c + 1]),
                             reads=[xt.r], writes=[junk.r, ss.r])
                    rstd_from_ss(ss, rs, nch, 1.0 / D)
                    for c in range(nch):
                        k.op("act", lambda e, c=c: e.activation(out=xt[:, c, :], in_=xt[:, c, :], func=AF.Copy,
                                                                scale=rs[:, c:c + 1]),
                             reads=[xt.r, rs.r], writes=[xt.r])
                    for kc in range(8):
                        pb = pA()

                        def tr(e, kc=kc, pb=pb):
                            for c in range(nch):
                                ii = e.transpose(out=pb[:, c * 128:(c + 1) * 128], in_=xt[:, c, kc * 128:(kc + 1) * 128],
                                                 identity=identf[:])
                            return ii
                        k.op("pe", tr, reads=[xt.r, identf.r], writes=[pb.r])
                        k.op("dve", lambda e, kc=kc, pb=pb: e.tensor_scalar(
                            out=hT[:, kc, 0:ntok], in0=pb[:, 0:ntok], scalar1=a1T[0][:, kc, r:r + 1],
                            scalar2=modT[0][:, kc, r:r + 1], op0=ALU.mult, op1=ALU.add),
                            reads=[pb.r, a1T[0].r, modT[0].r], writes=[hT.r])

                def proj(wt, c0, pb, hT, ntok):
                    def mm(e):
                        for kc in range(8):
                            ii = e.matmul(pb[:, 0:ntok], lhsT=wt[:, kc, c0:c0 + 128], rhs=hT[:, kc, 0:ntok],
                                          start=(kc == 0), stop=(kc == 7))
                        return ii
                    k.op("pe", mm, reads=[wt.r, hT.r], writes=[pb.r])

                def s_qk(i):
                    s, kind, tb, nch, ntok, r = meta(i)
                    hT = hTs[i % 2]
                    for which in (["k"] if kind == "ctx" else ["q", "k"]):
                        stg = qst if which == "q" else kst
                        for h in range(4):
                            c0 = (0 if which == "q" else 512) + h * 128
                            pa = pB()
                            proj(win, c0, pa, hT, ntok)
                            if kind == "ctx":
                                k.op("act", lambda e, pa=pa, h=h, stg=stg: e.copy(out=stg[:, h, 0:ntok], in_=pa[:, 0:ntok]),
                                     reads=[pa.r], writes=[stg.r])
                                continue
                            pr_ = pB()
                            proj(wrot, c0, pr_, hT, ntok)
                            t1 = t1s[ri[0] % 2]
                            t2 = t2s[ri[0] % 2]
                            ri[0] += 1
                            k.op("dve", lambda e, pa=pa, t1=t1: e.tensor_tensor(
                                out=t1[:], in0=pa[:], in1=cosT[:, tb * 512:(tb + 1) * 512], op=ALU.mult),
                                reads=[pa.r, cosT.r], writes=[t1.r])
                            k.op("dve", lambda e, pr_=pr_, t2=t2: e.tensor_tensor(
                                out=t2[:], in0=pr_[:], in1=sinT[:, tb * 512:(tb + 1) * 512], op=ALU.mult),
                                reads=[pr_.r, sinT.r], writes=[t2.r])
                            k.op("dve", lambda e, t1=t1, t2=t2, h=h, stg=stg: e.tensor_tensor(
                                out=stg[:, h, :], in0=t1[:], in1=t2[:], op=ALU.add),
                                reads=[t1.r, t2.r], writes=[stg.r])
                        if which == "q":
                            k.dma("sp", qTd[s].rearrange("h p t -> p h t")[:, :, tb * 512:(tb + 1) * 512], qst[:],
                                  [qst.r], [dummy])
                        else:
                            t0 = 0 if kind == "ctx" else CTX + tb * 512
                            k.dma("sp", kTd[s].rearrange("h p t -> p h t")[:, :, t0:t0 + ntok], kst[:, :, 0:ntok],
                                  [kst.r], [dummy])

                def s_vconv(i):
                    s, kind, tb, nch, ntok, r = meta(i)
                    hT = hTs[i % 2]
                    for c in range(nch):
                        pv = pC()

                        def mmv(e, c=c, pv=pv):
                            for kc in range(8):
                                ii = e.matmul(pv[:], lhsT=hT[:, kc, c * 128:(c + 1) * 128], rhs=win[:, kc, 1024:1536],
                                              start=(kc == 0), stop=(kc == 7))
                            return ii
                        k.op("pe", mmv, reads=[win.r, hT.r], writes=[pv.r])
                        k.op("act", lambda e, c=c, pv=pv: e.copy(out=vst[:, c, :], in_=pv[:]), reads=[pv.r], writes=[vst.r])
                    t0 = 0 if kind == "ctx" else CTX + tb * 512
                    k.dma("sp", vd[s][t0:t0 + ntok, :].rearrange("(c p) v -> p c v", p=128), vst[:, 0:nch, :],
                          [vst.r], [dummy])
                    if kind == "ctx":
                        return
                    for cc in range(4):
                        pgb = pC()
                        proj(win, 1536 + cc * 128, pgb, hT, ntok)
                        k.op("act", lambda e, cc=cc, pgb=pgb: e.copy(out=gbst[:, cc, :], in_=pgb[:]),
                             reads=[pgb.r], writes=[gbst.r])
                        pgc = pC()
                        proj(win, 2048 + cc * 128, pgc, hT, ntok)
                        gc_ = gcsb[cc % 2]
                        k.op("act", lambda e, pgc=pgc, gc_=gc_: e.copy(out=gc_[:], in_=pgc[:]), reads=[pgc.r], writes=[gc_.r])
                        pxs = pC()
                        proj(win, 2560 + cc * 128, pxs, hT, ntok)
                        k.op("dve", lambda e, cc=cc, pxs=pxs, gc_=gc_: e.tensor_tensor(
                            out=ust[:, cc, :], in0=pxs[:], in1=gc_[:], op=ALU.mult),
                            reads=[pxs.r, gc_.r], writes=[ust.r])
                    k.dma("sp", gbd[s].rearrange("(c p) t -> p c t", p=128)[:, :, tb * 512:(tb + 1) * 512], gbst[:],
                          [gbst.r], [dummy])
                    k.dma("sp", ud[s].rearrange("(c p) t -> p c t", p=128)[:, :, tb * 512:(tb + 1) * 512], ust[:],
                          [ust.r], [dummy])

                run_skewed(len(work), [load, s_norm, s_qk, s_vconv])
                k.barrier()

        def phase_l0_conv():
            with contextlib.ExitStack() as st:
                A = lambda shape, dt, name=None: alloc(st, shape, dt, name)
                ups = [A([128, SEQ + 2], F32, "upad") for _ in range(2)]
                gbs = [A([128, SEQ], F32, "gbs") for _ in range(2)]
                acc = [A([128, SEQ], F32, "cacc") for _ in range(2)]
                mst = [A([128, SEQ], BF16, "cmix") for _ in range(2)]
                for u_ in ups:
                    k.op("pool", lambda e, u_=u_: e.memset(u_[:, 0:1], 0.0), writes=[u_.r])
                    k.op("pool", lambda e, u_=u_: e.memset(u_[:, SEQ + 1:SEQ + 2], 0.0), reads=[u_.r], writes=[u_.r])
                i = 0
                for s in range(NS):
                    for cc in range(4):
                        up, gb, ac, ms = ups[i % 2], gbs[i % 2], acc[i % 2], mst[i % 2]
                        i += 1
                        k.dma("sp", up[:, 1:SEQ + 1], ud[s][cc * 128:(cc + 1) * 128, :], [], [up.r])
                        k.dma("sp", gb[:], gbd[s][cc * 128:(cc + 1) * 128, :], [], [gb.r])
                        k.op("act", lambda e, up=up, ac=ac, cc=cc: e.activation(
                            out=ac[:], in_=up[:, 0:SEQ], func=AF.Copy, scale=cwT[:, 0, cc:cc + 1]),
                            reads=[up.r, cwT.r], writes=[ac.r])
                        for tap in (1, 2):
                            k.op("dve", lambda e, up=up, ac=ac, cc=cc, tap=tap: e.scalar_tensor_tensor(
                                out=ac[:], in0=up[:, tap:tap + SEQ], scalar=cwT[:, tap, cc:cc + 1], in1=ac[:],
                                op0=ALU.mult, op1=ALU.add), reads=[up.r, cwT.r, ac.r], writes=[ac.r])
                        k.op("dve", lambda e, ac=ac, gb=gb, ms=ms: e.tensor_tensor(out=ms[:], in0=ac[:], in1=gb[:],
                                                                                    op=ALU.mult),
                             reads=[ac.r, gb.r], writes=[ms.r])
                        k.dma("sp", mixTd[s][512 + cc * 128:512 + (cc + 1) * 128, :], ms[:], [ms.r], [dummy])
                k.barrier()

        def phase_l0_attn():
            with contextlib.ExitStack() as st:
                A = lambda shape, dt, name=None: alloc(st, shape, dt, name)
                NKC = NKV // 128
                qTs = [A([128, SEQ], BF16, "qT") for _ in range(2)]
                kTs = [A([128, NKV], BF16, "kT") for _ in range(2)]
                vas = [A([128, NKC, 130], BF16, "vaug") for _ in range(2)]
                PTs = [A([128, NKC, 1024], BF16, "PT") for _ in range(2)]
                osb = [[A([128, 4, 130], F32, "osb") for _ in range(2)] for _ in range(3)]
                mixst = [A([128, SEQ], BF16, "mixst") for _ in range(2)]
                sublnb = A([128, 128], F32, "sublnb")
                slsc = A([128, 1], F32, "slsc")
                rec = [A([128, 2, 4], F32, "rec") for _ in range(3)]
                ass = [A([128, 4], F32, "ass") for _ in range(3)]
                ars = [A([128, 4], F32, "ars") for _ in range(3)]
                tt = [A([128, 128], F32, "att") for _ in range(2)]
                aa = [A([128, 4, 128], F32, "aa") for _ in range(3)]
                ajunk = A([128, 128], BF16, "ajunk")
                attb = [A([128, 128], BF16, "attb") for _ in range(2)]
                k.op("dve", lambda e: e.tensor_scalar(out=slsc[:], in0=slT[:], scalar1=1.0 - LAM_INIT, scalar2=None,
                                                      op0=ALU.mult), reads=[slT.r], writes=[slsc.r])
                bcast(sublnb, lambda j: slsc[:, 0:1], 1, slsc.r)
                for va in vas:
                    k.op("pool", lambda e, va=va: e.memset(va[:, :, 128:129], 1.0), writes=[va.r])
                    k.op("pool", lambda e, va=va: e.memset(va[:, :, 129:130], 0.0), reads=[va.r], writes=[va.r])
                ups = [A([128, SEQ + 2], F32, "upad") for _ in range(2)]
                gbs = [A([128, SEQ], F32, "gbs") for _ in range(2)]
                cacc = [A([128, SEQ], F32, "cacc") for _ in range(2)]
                cmst = [A([128, SEQ], BF16, "cmix") for _ in range(2)]
                for u_ in ups:
                    k.op("pool", lambda e, u_=u_: e.memset(u_[:, 0:1], 0.0), writes=[u_.r])
                    k.op("pool", lambda e, u_=u_: e.memset(u_[:, SEQ + 1:SEQ + 2], 0.0), reads=[u_.r], writes=[u_.r])

                def conv_item(ci):
                    s, cc = ci // 4, ci % 4
                    up, gb, ac, ms = ups[ci % 2], gbs[ci % 2], cacc[ci % 2], cmst[ci % 2]
                    k.dma("sp", up[:, 1:SEQ + 1], ud[s][cc * 128:(cc + 1) * 128, :], [], [up.r])
                    k.dma("sp", gb[:], gbd[s][cc * 128:(cc + 1) * 128, :], [], [gb.r])
                    k.op("act", lambda e: e.activation(out=ac[:], in_=up[:, 0:SEQ], func=AF.Copy, scale=cwT[:, 0, cc:cc + 1]),
                         reads=[up.r, cwT.r], writes=[ac.r])
                    for tap in (1, 2):
                        k.op("dve", lambda e, tap=tap: e.scalar_tensor_tensor(
                            out=ac[:], in0=up[:, tap:tap + SEQ], scalar=cwT[:, tap, cc:cc + 1], in1=ac[:],
                            op0=ALU.mult, op1=ALU.add), reads=[up.r, cwT.r, ac.r], writes=[ac.r])
                    k.op("dve", lambda e: e.tensor_tensor(out=ms[:], in0=ac[:], in1=gb[:], op=ALU.mult),
                         reads=[ac.r, gb.r], writes=[ms.r])
                    k.dma("sp", mixTd[s][512 + cc * 128:512 + (cc + 1) * 128, :], ms[:], [ms.r], [dummy])

                heads = [(s, h) for s in range(NS) for h in range(4)]
                steps = [(hi, Q) for hi in range(len(heads)) for Q in range(4)]
                NST = len(steps)
                sbank = [0]
                abank = [0]

                def load_head(hi):
                    s, h = heads[hi]
                    k.dma("sp", qTs[hi % 2][:], qTd[s][h], [], [qTs[hi % 2].r])
                    k.dma("sp", kTs[hi % 2][:], kTd[s][h], [], [kTs[hi % 2].r])
                    k.dma("sp", vas[hi % 2][:, :, 0:128],
                          vd[s][:, h * 128:(h + 1) * 128].rearrange("(c p) v -> p c v", p=128), [], [vas[hi % 2].r])

                def units_S(si):
                    hi, Q = steps[si]
                    qT, kT, PT = qTs[hi % 2], kTs[hi % 2], PTs[si % 2]
                    out = []
                    for c in range(NKC):
                        def u(c=c):
                            if c == 0 and Q == 0:
                                load_head(hi)
                            d = sbank[0] % 3
                            sbank[0] += 1
                            b0, b1 = ps[2 * d], ps[2 * d + 1]

                            def mm(e):
                                e.matmul(b0[:], lhsT=kT[0:64, c * 128:(c + 1) * 128], rhs=qT[0:64, Q * 512:(Q + 1) * 512],
                                         start=True, stop=True)
                                return e.matmul(b1[:], lhsT=kT[64:128, c * 128:(c + 1) * 128],
                                                rhs=qT[64:128, Q * 512:(Q + 1) * 512], start=True, stop=True)
                            k.op("pe", mm, reads=[kT.r, qT.r], writes=[b0.r, b1.r])
                            k.op("act", lambda e: e.activation(out=PT[:, c, :], in_=psd[d][:], func=AF.Exp, scale=0.125),
                                 reads=[b0.r, b1.r], writes=[PT.r])
                        out.append(u)
                    return out

                def units_AV(si):
                    hi, Q = steps[si]
                    PT, va = PTs[si % 2], vas[hi % 2]
                    out = []
                    for m in range(2):
                        ob = osb[si % 3][m]
                        for j in range(4):
                            def u(m=m, j=j, ob=ob):
                                pb = ps[6 + abank[0] % 2]
                                abank[0] += 1

                                def mm(e):
                                    for c in range(NKC):
                                        ii = e.matmul(pb[:, 0:130], lhsT=PT[:, c, m * 512 + j * 128:m * 512 + (j + 1) * 128],
                                                      rhs=va[:, c, :], start=(c == 0), stop=(c == NKC - 1))
                                    return ii
                                k.op("pe", mm, reads=[PT.r, va.r], writes=[pb.r])
                                k.op("dve", lambda e: e.tensor_copy(out=ob[:, j, :], in_=pb[:, 0:130]),
                                     reads=[pb.r], writes=[ob.r])
                            out.append(u)
                    return out

                def st_C1(si):
                    o1, o2 = osb[si % 3][0], osb[si % 3][1]
                    rc, as_, a_ = rec[si % 3], ass[si % 3], aa[si % 3]
                    k.op("dve", lambda e: e.reciprocal(out=rc[:, 0, :], in_=o1[:, :, 128]), reads=[o1.r], writes=[rc.r])
                    k.op("dve", lambda e: e.reciprocal(out=rc[:, 1, :], in_=o2[:, :, 128]), reads=[o2.r, rc.r], writes=[rc.r])
                    k.op("dve", lambda e: e.tensor_scalar(out=rc[:, 1, :], in0=rc[:, 1, :], scalar1=lamn[:, 0:1],
                                                          scalar2=None, op0=ALU.mult), reads=[rc.r, lamn.r], writes=[rc.r])
                    for j in range(4):
                        t_ = tt[j % 2]
                        k.op("dve", lambda e, j=j, t_=t_: e.tensor_scalar(out=t_[:], in0=o2[:, j, 0:128],
                                                                           scalar1=rc[:, 1, j:j + 1], scalar2=None,
                                                                           op0=ALU.mult),
                             reads=[o2.r, rc.r], writes=[t_.r])
                        k.op("dve", lambda e, j=j, t_=t_: e.scalar_tensor_tensor(
                            out=a_[:, j, :], in0=o1[:, j, 0:128], scalar=rc[:, 0, j:j + 1], in1=t_[:],
                            op0=ALU.mult, op1=ALU.add), reads=[o1.r, rc.r, t_.r], writes=[a_.r])
                        k.op("act", lambda e, j=j: e.activation(out=ajunk[:], in_=a_[:, j, :], func=AF.Square,
                                                                accum_out=as_[:, j:j + 1]),
                             reads=[a_.r], writes=[ajunk.r, as_.r])

                def st_C2(si):
                    rstd_from_ss(ass[si % 3], ars[si % 3], 4, 1.0 / 128)

                def st_C3(si):
                    hi, Q = steps[si]
                    s, h = heads[hi]
                    a_, ar = aa[si % 3], ars[si % 3]
                    ms = mixst[hi % 2]
                    for j in range(4):
                        ab = attb[j % 2]
                        k.op("dve", lambda e, j=j, ab=ab: e.scalar_tensor_tensor(
                            out=ab[:], in0=a_[:, j, :], scalar=ar[:, j:j + 1], in1=sublnb[:], op0=ALU.mult,
                            op1=ALU.mult), reads=[a_.r, ar.r, sublnb.r], writes=[ab.r])
                        pb = ps[6 + abank[0] % 2]
                        abank[0] += 1
                        pbb = pb[:].bitcast(BF16)
                        k.op("pe", lambda e, ab=ab, pbb=pbb: e.transpose(out=pbb[:, 0:128], in_=ab[:], identity=identb[:]),
                             reads=[ab.r, identb.r], writes=[pb.r])
                        k.op("dve", lambda e, j=j, pbb=pbb: e.tensor_copy(
                            out=ms[:, Q * 512 + j * 128:Q * 512 + (j + 1) * 128], in_=pbb[:, 0:128]),
                            reads=[pb.r], writes=[ms.r])
                    if Q == 3:
                        k.dma("sp", mixTd[s][h * 128:(h + 1) * 128, :], ms[:], [ms.r], [dummy])

                for t in range(NST + 4):
                    for fn, off in ((st_C3, 4), (st_C2, 3), (st_C1, 2)):
                        if 0 <= t - off < NST:
                            fn(t - off)
                    if t % 4 == 1 and t // 4 < 8:
                        conv_item(t // 4)
                    us_ = units_S(t) if t < NST else []
                    ua_ = units_AV(t - 1) if 0 <= t - 1 < NST else []
                    ai = 0
                    for ci, u in enumerate(us_):
                        u()
                        while ai < len(ua_) and (ai + 1) * len(us_) <= (ci + 1) * len(ua_) + len(ua_) - 1:
                            ua_[ai]()
                            ai += 1
                    while ai < len(ua_):
                        ua_[ai]()
                        ai += 1
                k.barrier()

        class TailBufs:
            pass

        def tail_setup(st, l):
            A = lambda shape, dt, name=None: alloc(st, shape, dt, name)
            tb = TailBufs()
            tb.wout = A([128, 8, D], BF16, "wout")
            k.dma("pool", tb.wout[:], w_out[l].rearrange("(j p) n -> p j n", p=128), [], [tb.wout.r])
            tb.g1b = [A([128, D], F32, "g1b") for _ in range(NS)]
            tb.a2b = [A([128, D], F32, "a2b") for _ in range(NS)]
            tb.sh2b = [A([128, D], F32, "sh2b") for _ in range(NS)]
            for s in range(NS):
                bcast(tb.g1b[s], lambda j, s=s: modT[l][:, 16 + j, s:s + 1], 8, modT[l].r)
                bcast(tb.a2b[s], lambda j, s=s: a2T[l][:, j, s:s + 1], 8, a2T[l].r)
                bcast(tb.sh2b[s], lambda j, s=s: modT[l][:, 24 + j, s:s + 1], 8, modT[l].r)
            tb.tmp = [A([128, D], F32, "ttmp") for _ in range(2)]
            tb.xnew = [A([128, D], F32, "xnew") for _ in range(3)]
            tb.h2f = [A([128, D], F32, "h2f") for _ in range(2)]
            tb.h2T = [A([128, 8, 128], F32, "h2T") for _ in range(2)]
            tb.junk = A([128, D], BF16, "tjunk")
            tb.ss = [A([128, 1], F32, "tss") for _ in range(4)]
            tb.rs = [A([128, 1], F32, "trs") for _ in range(4)]
            tb.mx = [A([128, 1], F32, "tmx") for _ in range(3)]
            tb.es = [A([128, 1], F32, "tes") for _ in range(3)]
            tb.ex = [A([128, NE], F32, "tex") for _ in range(3)]
            tb.pr2 = [A([128, 64], F32, "pr2") for _ in range(2)]
            for p_ in tb.pr2:
                k.op("pool", lambda e, p_=p_: e.memset(p_[:], 0.0), writes=[p_.r])
            return tb

        def tail_stages(tb, l, work, mix_of, xt_of, pools):
            R4, R3, R2 = 4, 3, 2

            def t1(i):
                ch, s = work[i]
                mix_lhs, mixres = mix_of(i)
                xt_ap, xtres = xt_of(i)
                tmp, xnew, ss = tb.tmp[i % 2], tb.xnew[i % R3], tb.ss[i % R4]
                for half in range(2):
                    pb = pools[0]()

                    def mm(e, half=half, pb=pb):
                        for kc in range(8):
                            ii = e.matmul(pb[:], lhsT=mix_lhs(kc), rhs=tb.wout[:, kc, half * 512:(half + 1) * 512],
                                          start=(kc == 0), stop=(kc == 7))
                        return ii
                    k.op("pe", mm, reads=[mixres, tb.wout.r], writes=[pb.r])
                    k.op("dve", lambda e, half=half, pb=pb: e.tensor_tensor(
                        out=tmp[:, half * 512:(half + 1) * 512], in0=pb[:], in1=tb.g1b[s][:, half * 512:(half + 1) * 512],
                        op=ALU.mult), reads=[pb.r, tb.g1b[s].r], writes=[tmp.r])
                k.op("dve", lambda e: e.tensor_tensor(out=xnew[:], in0=tmp[:], in1=xt_ap, op=ALU.add),
                     reads=[tmp.r, xtres], writes=[xnew.r])
                k.dma("sp", xd[s][ch * 128:(ch + 1) * 128, :], xnew[:], [xnew.r], [dummy])
                k.op("act", lambda e: e.activation(out=tb.junk[:], in_=xnew[:], func=AF.Square, accum_out=ss[:, 0:1]),
                     reads=[xnew.r], writes=[tb.junk.r, ss.r])

            def t2(i):
                rstd_from_ss(tb.ss[i % R4], tb.rs[i % R4], 1, 1.0 / D)

            def t3(i):
                ch, s = work[i]
                xnew, rs, h2f, h2T = tb.xnew[i % R3], tb.rs[i % R4], tb.h2f[i % R2], tb.h2T[i % R2]
                k.op("dve", lambda e: e.scalar_tensor_tensor(out=h2f[:], in0=xnew[:], scalar=rs[:, 0:1], in1=tb.a2b[s][:],
                                                             op0=ALU.mult, op1=ALU.mult),
                     reads=[xnew.r, rs.r, tb.a2b[s].r], writes=[h2f.r])
                k.op("dve", lambda e: e.tensor_tensor(out=h2f[:], in0=h2f[:], in1=tb.sh2b[s][:], op=ALU.add),
                     reads=[h2f.r, tb.sh2b[s].r], writes=[h2f.r])
                k.dma("pool", h2d[s][ch * 128:(ch + 1) * 128, :], h2f[:], [h2f.r], [dummy])
                for q in range(2):
                    pb = pools[1]()

                    def tr(e, q=q, pb=pb):
                        for c in range(4):
                            kc = q * 4 + c
                            ii = e.transpose(out=pb[:, c * 128:(c + 1) * 128], in_=h2f[:, kc * 128:(kc + 1) * 128],
                                             identity=identf[:])
                        return ii
                    k.op("pe", tr, reads=[h2f.r, identf.r], writes=[pb.r])
                    k.op("act", lambda e, q=q, pb=pb: e.copy(out=h2T[:, q * 4:(q + 1) * 4, :],
                                                             in_=pb[:].rearrange("p (c t) -> p c t", c=4)),
                         reads=[pb.r], writes=[h2T.r])

            def t4(i):
                h2T, mx, es, ex = tb.h2T[i % R2], tb.mx[i % R3], tb.es[i % R3], tb.ex[i % R3]
                pl = pools[2]()

                def mmr(e):
                    for kc in range(8):
                        ii = e.matmul(pl[:, 0:NE], lhsT=h2T[:, kc, :], rhs=wr[l][:, kc, :], start=(kc == 0), stop=(kc == 7))
                    return ii
                k.op("pe", mmr, reads=[h2T.r, wr[l].r], writes=[pl.r])
                k.op("dve", lambda e: e.tensor_reduce(out=mx[:], in_=pl[:, 0:NE], axis=AX.X, op=ALU.max, negate=True),
                     reads=[pl.r], writes=[mx.r])
                k.op("act", lambda e: e.activation(out=ex[:], in_=pl[:, 0:NE], func=AF.Exp, bias=mx[:, 0:1], scale=1.0,
                                                   accum_out=es[:, 0:1]), reads=[pl.r, mx.r], writes=[ex.r, es.r])

            def t5(i):
                ch, s = work[i]
                es, ex = tb.es[i % R3], tb.ex[i % R3]
                pr2 = tb.pr2[ch % 2]
                k.op("dve", lambda e: e.reciprocal(out=es[:], in_=es[:]), reads=[es.r], writes=[es.r])
                k.op("dve", lambda e: e.tensor_scalar(out=pr2[:, s * 32:s * 32 + NE], in0=ex[:], scalar1=es[:, 0:1],
                                                      scalar2=None, op0=ALU.mult), reads=[ex.r, es.r], writes=[pr2.r])
                if s == NS - 1:
                    pt = pools[3]()
                    k.op("pe", lambda e: e.transpose(out=pt[0:64, 0:128], in_=pr2[:], identity=identf[:]),
                         reads=[pr2.r, identf.r], writes=[pt.r])
                    k.op("act", lambda e: e.copy(out=probsT[:, ch * 128:(ch + 1) * 128], in_=pt[0:64, 0:128]),
                         reads=[pt.r], writes=[probsT.r])
            return [t1, t2, t3, t4, t5]

        def phase_l0_tail():
            with contextlib.ExitStack() as st:
                A = lambda shape, dt, name=None: alloc(st, shape, dt, name)
                tb = tail_setup(st, 0)
                mixTs = [A([128, 8, 128], BF16, "mixT") for _ in range(3)]
                xts = [A([128, D], F32, "xt") for _ in range(3)]
                work = [(ch, s) for ch in range(16) for s in range(NS)]

                def s_load(i):
                    ch, s = work[i]
                    k.dma("sp", mixTs[i % 3][:], mixTd[s].rearrange("(j p) t -> p j t", p=128)[:, :, ch * 128:(ch + 1) * 128],
                          [], [mixTs[i % 3].r])
                    k.dma("sp", xts[i % 3][:], x_in[s][ch * 128:(ch + 1) * 128, :], [], [xts[i % 3].r])

                stages = tail_stages(tb, 0, work,
                                     lambda i: ((lambda kc, mT=mixTs[i % 3]: mT[:, kc, :]), mixTs[i % 3].r),
                                     lambda i: (xts[i % 3][:], xts[i % 3].r),
                                     [bank_pool([0, 1, 2]), bank_pool([3, 4]), bank_pool([5, 6]), bank_pool([7])])
                m1 = mod_items(1, st, 2, bank_pool([7]))

                def extra(t):
                    if t >= 2 and t % 2 == 0 and (t - 2) // 2 < len(m1):
                        m1[(t - 2) // 2]()
                run_skewed(len(work), [s_load] + stages, extra)
                k.barrier()

        def phase_topk():
            for r in range(CAP // 8):
                sl = slice(r * 8, (r + 1) * 8)
                k.op("dve", lambda e, sl=sl: e.max(out=tvals[:, sl], in_=probsT[:]), reads=[probsT.r], writes=[tvals.r])
                k.op("dve", lambda e, sl=sl: e.max_index(out=tidx[:, sl], in_max=tvals[:, sl], in_values=probsT[:]),
                     reads=[probsT.r, tvals.r], writes=[tidx.r])
                k.op("dve", lambda e, sl=sl: e.match_replace(out=probsT[:], in_to_replace=tvals[:, sl],
                                                             in_values=probsT[:], imm_value=-1.0),
                     reads=[probsT.r, tvals.r], writes=[probsT.r])
            k.op("dve", lambda e: e.tensor_copy(out=tidxf[:], in_=tidx[:]), reads=[tidx.r], writes=[tidxf.r])
            for half in range(2):
                pa = nps()
                k.op("pe", lambda e, pa=pa, half=half: e.transpose(out=pa[:, 0:64], in_=tidxf[:, half * 128:(half + 1) * 128],
                                                                   identity=identf[0:64, 0:64]),
                     reads=[tidxf.r, identf.r], writes=[pa.r])
                k.op("dve", lambda e, pa=pa, half=half: e.tensor_copy(out=idxT[:, half, :], in_=pa[:, 0:64]),
                     reads=[pa.r], writes=[idxT.r])
                pv = nps()
                k.op("pe", lambda e, pv=pv, half=half: e.transpose(out=pv[:, 0:64], in_=tvals[:, half * 128:(half + 1) * 128],
                                                                   identity=identf[0:64, 0:64]),
                     reads=[tvals.r, identf.r], writes=[pv.r])
                k.op("act", lambda e, pv=pv, half=half: e.copy(out=valT[:, half, :], in_=pv[:, 0:64]),
                     reads=[pv.r], writes=[valT.r])
            k.op("pool", lambda e: e.memset(probsT[:], 0.0), reads=[probsT.r], writes=[probsT.r])
            k.barrier()

        def phase_moe(l):
            with contextlib.ExitStack() as st:
                A = lambda shape, dt, name=None: alloc(st, shape, dt, name)
                wg = [A([128, 8, D], BF16, "wg") for _ in range(2)]
                wu = [A([128, 8, D], BF16, "wu") for _ in range(2)]
                wd = [A([128, 8, D], BF16, "wd") for _ in range(2)]
                xs = [[A([128, D], BF16, "xs") for _ in range(4)] for _ in range(2)]
                xsT = [A([128, 8, 512], BF16, "xsT") for _ in range(2)]
                hT = [A([128, 8, 512], BF16, "hT") for _ in range(2)]
                sg = [A([128, 512], F32, "sg") for _ in range(2)]
                ysb = [A([128, D], F32, "ysb") for _ in range(4)]
                g2b = [A([128, D], F32, "g2b") for _ in range(NS)]
                for s in range(NS):
                    bcast(g2b[s], lambda j, s=s: modT[l][:, 40 + j, s:s + 1], 8, modT[l].r)
                rx = [Res("xd_s%d" % s) for s in range(NS)]

                def prefetch_w(e):
                    b = e % 2
                    for wt, src in ((wg[b], w_gate), (wu[b], w_up), (wd[b], w_down)):
                        k.dma("pool", wt[:], src[l][e].rearrange("(j p) n -> p j n", p=128), [], [wt.r])

                def prefetch_g(e):
                    b = e % 2
                    for s in range(NS):
                        for half in range(2):
                            t = xs[b][s * 2 + half]
                            k.idma(t[:], None, h2d[s], bass.IndirectOffsetOnAxis(
                                ap=idxT[:, half, s * 32 + e:s * 32 + e + 1], axis=0), [idxT.r], [t.r])

                def prefetch(e):
                    if e >= 2:
                        prefetch_w(e)
                    prefetch_g(e)

                prefetch_w(0)
                prefetch_w(1)
                phase_topk()
                import os
                MODE = os.environ.get("MK_MOE", "")
                NEX = 1 if MODE == "ne1" else NE
                prefetch_g(0)
                sgi = 0
                for e in range(NEX):
                    b = e % 2
                    if e + 1 < NEX:
                        prefetch(e + 1)
                    if MODE == "nocomp":
                        for tc in range(4):
                            s, half = tc // 2, tc % 2
                            y = ysb[tc]
                            k.op("dve", lambda e_, y=y, tc=tc: e_.tensor_copy(out=y[:], in_=xs[b][tc][:]),
                                 reads=[xs[b][tc].r, wg[b].r, wu[b].r, wd[b].r], writes=[y.r])
                            k.idma(xd[s], bass.IndirectOffsetOnAxis(ap=idxT[:, half, s * 32 + e:s * 32 + e + 1], axis=0),
                                   y[:], None, [y.r, idxT.r, rx[s]], [rx[s]], compute_op=ALU.add)
                        continue
                    for tc in range(4):
                        t = xs[b][tc]
                        pb = nps()
                        pbb = pb[:].bitcast(BF16)

                        def tr(e_, t=t, pbb=pbb):
                            for kc in range(8):
                                ii = e_.transpose(out=pbb[:, kc * 128:(kc + 1) * 128], in_=t[:, kc * 128:(kc + 1) * 128],
                                                  identity=identb[:])
                            return ii
                        k.op("pe", tr, reads=[t.r, identb.r], writes=[pb.r])
                        k.op("act", lambda e_, tc=tc, pbb=pbb: e_.copy(out=xsT[b][:, :, tc * 128:(tc + 1) * 128],
                                                                      in_=pbb.rearrange("p (j t) -> p j t", j=8)),
                             reads=[pb.r], writes=[xsT[b].r])
                    for fc in range(8):
                        pg, pu = nps(), nps()
                        for wt, pb in ((wg[b], pg), (wu[b], pu)):
                            def mm(e_, wt=wt, pb=pb, fc=fc):
                                for kc in range(8):
                                    ii = e_.matmul(pb[:], lhsT=wt[:, kc, fc * 128:(fc + 1) * 128], rhs=xsT[b][:, kc, :],
                                                   start=(kc == 0), stop=(kc == 7))
                                return ii
                            k.op("pe", mm, reads=[wt.r, xsT[b].r], writes=[pb.r])
                        sg_ = sg[sgi % 2]
                        sgi += 1
                        k.op("act", lambda e_, pg=pg, sg_=sg_: e_.activation(out=sg_[:], in_=pg[:], func=AF.Silu),
                             reads=[pg.r], writes=[sg_.r])
                        k.op("dve", lambda e_, pu=pu, sg_=sg_, fc=fc: e_.tensor_tensor(out=hT[b][:, fc, :], in0=pu[:],
                                                                                     in1=sg_[:], op=ALU.mult),
                             reads=[pu.r, sg_.r], writes=[hT[b].r])
                    for tc in range(4):
                        s, half = tc // 2, tc % 2
                        y = ysb[tc]
                        for dh in range(2):
                            pb = nps()

                            def mmd(e_, tc=tc, dh=dh, pb=pb):
                                for fc in range(8):
                                    ii = e_.matmul(pb[:], lhsT=hT[b][:, fc, tc * 128:(tc + 1) * 128],
                                                   rhs=wd[b][:, fc, dh * 512:(dh + 1) * 512], start=(fc == 0), stop=(fc == 7))
                                return ii
                            k.op("pe", mmd, reads=[hT[b].r, wd[b].r], writes=[pb.r])
                            k.op("dve", lambda e_, pb=pb, dh=dh, y=y, s=s, half=half: e_.scalar_tensor_tensor(
                                out=y[:, dh * 512:(dh + 1) * 512], in0=pb[:], scalar=valT[:, half, s * 32 + e:s * 32 + e + 1],
                                in1=g2b[s][:, dh * 512:(dh + 1) * 512], op0=ALU.mult, op1=ALU.mult),
                                reads=[pb.r, valT.r, g2b[s].r], writes=[y.r])
                        k.idma(xd[s], bass.IndirectOffsetOnAxis(ap=idxT[:, half, s * 32 + e:s * 32 + e + 1], axis=0),
                               y[:], None, [y.r, idxT.r, rx[s]], [rx[s]], compute_op=ALU.add)
                k.barrier()

        def phase_l1():
            with contextlib.ExitStack() as st:
                A = lambda shape, dt, name=None: alloc(st, shape, dt, name)
                tb = tail_setup(st, 1)
                w1 = A([128, 8, 2048], BF16, "w1in")
                for cb in range(2):
                    k.dma("pool", w1[:, :, cb * 1024:(cb + 1) * 1024],
                          odd_w_in[:, cb * 1024:(cb + 1) * 1024].rearrange("(j p) n -> p j n", p=128), [], [w1.r])
                wsf = A([128, 4, 128], F32, "wsf")
                wsT = A([128, 4, 128], BF16, "wsT")
                k.dma("sp", wsf[:], odd_w_s.rearrange("g i j -> i g j"), [], [wsf.r])
                for g in range(4):
                    pb = nps()
                    k.op("pe", lambda e, g=g, pb=pb: e.transpose(out=pb[:, 0:128], in_=wsf[:, g, :], identity=identf[:]),
                         reads=[wsf.r, identf.r], writes=[pb.r])
                    k.op("act", lambda e, g=g, pb=pb: e.copy(out=wsT[:, g, :], in_=pb[:, 0:128]), reads=[pb.r], writes=[wsT.r])
                vnb = A([128, D], F32, "vnb")
                bcast(vnb, lambda j: vnT[:, j:j + 1], 8, vnT.r)
                NX = 4
                xt2 = [A([128, D], F32, "xt2") for _ in range(3)]
                xts = [A([128, D], F32, "xt") for _ in range(NX)]
                hTs = [A([128, 8, 128], BF16, "hT") for _ in range(2)]
                us = [A([128, D], F32, "u") for _ in range(3)]
                vs = [A([128, D], F32, "v") for _ in range(2)]
                vnn = [A([128, D], BF16, "vn") for _ in range(2)]
                mixs = [A([128, D], BF16, "mix") for _ in range(2)]
                mixTs = [A([128, 8, 128], BF16, "mixT") for _ in range(2)]
                junk = A([128, D], BF16, "junk")
                ss = [A([128, 1], F32, "ss") for _ in range(3)]
                rs = [A([128, 1], F32, "rs") for _ in range(3)]
                ss2 = [A([128, 1], F32, "ss2") for _ in range(3)]
                rs2 = [A([128, 1], F32, "rs2") for _ in range(3)]
                work = [(ch, s) for ch in range(16) for s in range(NS)]

                pl_tr = bank_pool([0, 1])
                pl_pj = bank_pool([2, 3])
                pl_sm = bank_pool([4])
                pl_mt = bank_pool([5])

                def s_load(i):
                    ch, s = work[i]
                    k.dma("sp", xts[i % NX][:], xd[s][ch * 128:(ch + 1) * 128, :], [], [xts[i % NX].r])

                def s_sq(i):
                    xt = xts[i % NX]
                    k.op("act", lambda e: e.activation(out=junk[:], in_=xt[:], func=AF.Square, accum_out=ss[i % 3][:, 0:1]),
                         reads=[xt.r], writes=[junk.r, ss[i % 3].r])

                def s_rstd(i):
                    rstd_from_ss(ss[i % 3], rs[i % 3], 1, 1.0 / D)

                def s_norm(i):
                    ch, s = work[i]
                    xt, hT = xts[i % NX], hTs[i % 2]
                    xn = xt
                    k.op("act", lambda e: e.activation(out=xn[:], in_=xt[:], func=AF.Copy, scale=rs[i % 3][:, 0:1]),
                         reads=[xt.r, rs[i % 3].r], writes=[xn.r])
                    for q in range(2):
                        pb = pl_tr()

                        def tr(e, q=q, pb=pb):
                            for c in range(4):
                                kc = q * 4 + c
                                ii = e.transpose(out=pb[:, c * 128:(c + 1) * 128], in_=xn[:, kc * 128:(kc + 1) * 128],
                                                 identity=identf[:])
                            return ii
                        k.op("pe", tr, reads=[xn.r, identf.r], writes=[pb.r])
                        for c in range(4):
                            kc = q * 4 + c
                            k.op("dve", lambda e, kc=kc, c=c, pb=pb: e.tensor_scalar(
                                out=hT[:, kc, :], in0=pb[:, c * 128:(c + 1) * 128], scalar1=a1T[1][:, kc, s:s + 1],
                                scalar2=modT[1][:, kc, s:s + 1], op0=ALU.mult, op1=ALU.add),
                                reads=[pb.r, a1T[1].r, modT[1].r], writes=[hT.r])

                def s_proj(i):
                    hT, u_, v_ = hTs[i % 2], us[i % 3], vs[i % 2]
                    for cb in range(4):
                        pb = pl_pj()

                        def mm(e, cb=cb, pb=pb):
                            for kc in range(8):
                                ii = e.matmul(pb[:], lhsT=hT[:, kc, :], rhs=w1[:, kc, cb * 512:(cb + 1) * 512],
                                              start=(kc == 0), stop=(kc == 7))
                            return ii
                        k.op("pe", mm, reads=[hT.r, w1.r], writes=[pb.r])
                        dst = u_ if cb < 2 else v_
                        k.op("act", lambda e, cb=cb, pb=pb, dst=dst: e.activation(
                            out=dst[:, (cb % 2) * 512:(cb % 2 + 1) * 512], in_=pb[:], func=AF.Gelu),
                            reads=[pb.r], writes=[dst.r])
                    k.op("act", lambda e: e.activation(out=junk[:], in_=v_[:], func=AF.Square, accum_out=ss2[i % 3][:, 0:1]),
                         reads=[v_.r], writes=[junk.r, ss2[i % 3].r])

                def s_rstd2(i):
                    rstd_from_ss(ss2[i % 3], rs2[i % 3], 1, 1.0 / D)

                def s_gate(i):
                    u_, v_, vn_, mix = us[i % 3], vs[i % 2], vnn[i % 2], mixs[i % 2]
                    k.op("dve", lambda e: e.scalar_tensor_tensor(out=vn_[:], in0=v_[:], scalar=rs2[i % 3][:, 0:1], in1=vnb[:],
                                                                 op0=ALU.mult, op1=ALU.mult),
                         reads=[v_.r, rs2[i % 3].r, vnb.r], writes=[vn_.r])
                    for g2_ in range(2):
                        pb = pl_sm()

                        def mms(e, g2_=g2_, pb=pb):
                            for gg in range(2):
                                g = g2_ * 2 + gg
                                ii = e.matmul(pb[:, gg * 256:(gg + 1) * 256], lhsT=wsT[:, g, :],
                                              rhs=vn_[:, g * 256:(g + 1) * 256], start=True, stop=True)
                            return ii
                        k.op("pe", mms, reads=[wsT.r, vn_.r], writes=[pb.r])
                        for gg in range(2):
                            g = g2_ * 2 + gg
                            k.op("dve", lambda e, g=g, gg=gg, pb=pb: e.scalar_tensor_tensor(
                                out=mix[:, g * 256:(g + 1) * 256], in0=pb[:, gg * 256:(gg + 1) * 256],
                                scalar=bsT[:, g:g + 1], in1=u_[:, g * 256:(g + 1) * 256], op0=ALU.add, op1=ALU.mult),
                                reads=[pb.r, bsT.r, u_.r], writes=[mix.r])

                def s_mixT(i):
                    mix, mixT = mixs[i % 2], mixTs[i % 2]
                    pb = pl_mt()
                    pbb = pb[:].bitcast(BF16)

                    def trm(e, pbb=pbb):
                        for kc in range(8):
                            ii = e.transpose(out=pbb[:, kc * 128:(kc + 1) * 128], in_=mix[:, kc * 128:(kc + 1) * 128],
                                             identity=identb[:])
                        return ii
                    k.op("pe", trm, reads=[mix.r, identb.r], writes=[pb.r])
                    k.op("act", lambda e, pbb=pbb: e.copy(out=mixT[:], in_=pbb.rearrange("p (j t) -> p j t", j=8)),
                         reads=[pb.r], writes=[mixT.r])

                def s_load2(i):
                    ch, s = work[i]
                    k.dma("sp", xt2[i % 3][:], xd[s][ch * 128:(ch + 1) * 128, :], [], [xt2[i % 3].r])

                stages = tail_stages(tb, 1, work,
                                     lambda i: ((lambda kc, mT=mixTs[i % 2]: mT[:, kc, :]), mixTs[i % 2].r),
                                     lambda i: (xt2[i % 3][:], xt2[i % 3].r),
                                     [bank_pool([6, 7]), pl_tr, pl_sm, pl_sm])
                run_skewed(len(work), [s_load, s_sq, s_rstd, s_norm, s_proj, s_rstd2, s_gate, s_load2, s_mixT] + stages)
                k.barrier()

        def phase_final(src):
            with contextlib.ExitStack() as st:
                A = lambda shape, dt, name=None: alloc(st, shape, dt, name)
                fnb = A([128, D], F32, "fnb")
                bcast(fnb, lambda j: fnT[:, j:j + 1], 8, fnT.r)
                xts = [A([128, 4, D], F32, "xt") for _ in range(2)]
                ots = [A([128, 4, D], F32, "ot") for _ in range(2)]
                junk = A([128, D], BF16, "junk")
                ss = [A([128, 4], F32, "ss") for _ in range(2)]
                rs = [A([128, 4], F32, "rs") for _ in range(2)]
                work = [(s, tb) for s in range(NS) for tb in range(4)]

                def load(i):
                    s, tb = work[i]
                    k.dma("sp", xts[i % 2][:], src[s][tb * 512:(tb + 1) * 512, :].rearrange("(c p) d -> p c d", p=128),
                          [], [xts[i % 2].r])
                load(0)
                for i, (s, tb) in enumerate(work):
                    if i + 1 < len(work):
                        load(i + 1)
                    xt, ot = xts[i % 2], ots[i % 2]
                    for c in range(4):
                        k.op("act", lambda e, c=c: e.activation(out=junk[:], in_=xt[:, c, :], func=AF.Square,
                                                                accum_out=ss[i % 2][:, c:c + 1]),
                             reads=[xt.r], writes=[junk.r, ss[i % 2].r])
                    rstd_from_ss(ss[i % 2], rs[i % 2], 4, 1.0 / D)
                    for c in range(4):
                        k.op("dve", lambda e, c=c: e.scalar_tensor_tensor(
                            out=ot[:, c, :], in0=xt[:, c, :], scalar=rs[i % 2][:, c:c + 1], in1=fnb[:], op0=ALU.mult,
                            op1=ALU.mult), reads=[xt.r, rs[i % 2].r, fnb.r], writes=[ot.r])
                    k.dma("sp", out_d[s][tb * 512:(tb + 1) * 512, :].rearrange("(c p) d -> p c d", p=128), ot[:],
                          [ot.r], [dummy])
                k.barrier()

        stages = ["l0_inproj", "l0_attn", "l0_tail", "moe0", "l1", "moe1"]
        fns = {"l0_inproj": phase_l0_inproj, "l0_conv": phase_l0_conv, "l0_attn": phase_l0_attn,
               "l0_tail": phase_l0_tail, "topk0": phase_topk, "moe0": lambda: phase_moe(0), "l1": phase_l1,
               "topk1": phase_topk, "moe1": lambda: phase_moe(1)}
        k.barrier()
        for sname in stages:
            fns[sname]()
            if stop == sname:
                break
        if stop in ("all", "moe1"):
            phase_final(xd)
        elif stop in ("l0_tail", "topk0", "moe0", "l1", "topk1"):
            k.dma("sp", dbg_idx, idxT[:], [idxT.r], [dummy])
            k.dma("sp", dbg_val, valT[:], [valT.r], [dummy])
            with contextlib.ExitStack() as st:
                xt = alloc(st, [128, 4, D], F32, "dbg")
                for s in range(NS):
                    for tb in range(4):
                        k.dma("sp", xt[:], xd[s][tb * 512:(tb + 1) * 512, :].rearrange("(c p) d -> p c d", p=128), [], [xt.r])
                        k.dma("sp", out_d[s][tb * 512:(tb + 1) * 512, :].rearrange("(c p) d -> p c d", p=128), xt[:],
                              [xt.r], [dummy])
                k.barrier()
        k.finish()
    print("built: %d instructions, %d waits" % (k.n_inst, k.n_wait))
    return nc


def _rope_tables():
    rows = SEQ // 64
    row = np.repeat(np.arange(rows), 64).astype(np.float32)
    col = np.tile(np.arange(64), rows).astype(np.float32)
    half = 32
    inv = (1.0 / (10000.0 ** (np.arange(0, half, 2, dtype=np.float32) / half))).astype(np.float32)
    ang_r = row[:, None] * inv
    ang_c = col[:, None] * inv
    ang = np.concatenate([ang_r, ang_r, ang_c, ang_c], axis=-1)
    cosT = np.ascontiguousarray(np.tile(np.cos(ang).T, (2, 1))).astype(np.float32)
    sinT = np.ascontiguousarray(np.tile(np.sin(ang).T, (2, 1))).astype(np.float32)
    return cosT, sinT


_NC_CACHE = {}


def kernel(x, c, ctx, c_ctx, w_mod, b_mod, norm1, norm2, even_w_in, even_lambda, even_subln, even_conv_w,
           odd_w_in, odd_v_norm, odd_w_s, odd_b_s, w_out, w_router, w_gate, w_up, w_down, final_norm, _stop="all"):
    f = lambda a: np.ascontiguousarray(np.asarray(a, dtype=np.float32))
    if _stop not in _NC_CACHE:
        _NC_CACHE[_stop] = build_nc(_stop)
    nc = _NC_CACHE[_stop]
    cosT, sinT = _rope_tables()
    x, c, ctx, c_ctx = f(x), f(c), f(ctx), f(c_ctx)
    shared = {
        "w_mod": f(w_mod), "b_mod": f(b_mod), "norm1": f(norm1), "norm2": f(norm2),
        "even_w_in": f(even_w_in)[0], "even_lambda": f(even_lambda)[0], "even_subln": f(even_subln)[0],
        "even_conv_w": f(even_conv_w)[0], "odd_w_in": f(odd_w_in)[0], "odd_v_norm": f(odd_v_norm)[0],
        "odd_w_s": f(odd_w_s)[0], "odd_b_s": f(odd_b_s)[0], "w_out": f(w_out), "w_router": f(w_router),
        "w_gate": f(w_gate), "w_up": f(w_up), "w_down": f(w_down), "final_norm": f(final_norm),
        "rope_cos": cosT, "rope_sin": sinT,
    }
    in_maps = []
    for i in range(N_CORES):
        sl = slice(i * NS, (i + 1) * NS)
        m = dict(shared)
        m["x"] = np.ascontiguousarray(x[sl])
        m["ctx"] = np.ascontiguousarray(ctx[sl])
        m["cvec"] = np.ascontiguousarray(np.stack([c[i * NS], c[i * NS + 1], c_ctx, c_ctx], axis=0))
        in_maps.append(m)
    res = run_bass_kernel_spmd(nc, in_maps, core_ids=list(range(N_CORES)))
    if _stop != "all":
        global _DBG
        _DBG = res.results
    return np.concatenate([r["out"] for r in res.results], axis=0).astype(np.float32)
```

```python
import contextlib
import math
import numpy as np
import concourse.bass as bass
import concourse.mybir as mybir
from concourse.bass_utils import run_bass_kernel_spmd

F32 = mybir.dt.float32
BF16 = mybir.dt.bfloat16
U32 = mybir.dt.uint32
I32 = mybir.dt.int32
ALU = mybir.AluOpType
AF = mybir.ActivationFunctionType
AX = mybir.AxisListType

N_CORES = 8
D = 1024
SEQ = 2048
CTX = 256
NS = 2
NE = 16
CAP = 256
EPS = 1e-6


class Res:
    __slots__ = ("name", "w", "r", "dsem")

    def __init__(self, name):
        self.name = name
        self.w = None
        self.r = {}
        self.dsem = None


class K:
    def __init__(self, nc, n_dma_sems=96, same_engine_sync=True):
        self.nc = nc
        self.stack = contextlib.ExitStack()
        self.eng = {"pe": nc.tensor, "act": nc.scalar, "dve": nc.vector, "pool": nc.gpsimd, "sp": nc.sync}
        self.sems = {}
        self.cnt = {}
        for e in self.eng:
            self.sems[e] = self.stack.enter_context(nc.semaphore("sem_" + e))
            self.cnt[e] = 0
        self.dsems = {"hw": [], "sw": []}
        self.dsem_i = {"hw": 0, "sw": 0}
        for i in range(n_dma_sems):
            k = "d%d" % i
            self.sems[k] = self.stack.enter_context(nc.semaphore("sem_" + k))
            self.cnt[k] = 0
            self.dsems["sw" if i % 3 == 2 else "hw"].append(k)
        self.waited = {e: {} for e in self.eng}
        self.same_engine_sync = same_engine_sync
        self.n_inst = 0
        self.n_wait = 0

    def _deps(self, reads, writes, eng=None):
        deps = {}
        def add(ev):
            if ev is None:
                return
            k, v = ev
            if deps.get(k, 0) < v:
                deps[k] = v
        for r in reads:
            add(r.w)
        for w in writes:
            if not (eng is not None and w.w is not None and w.w[0] == eng):
                add(w.w)
            for k, v in w.r.items():
                add((k, v))
        return deps

    def _wait(self, e, deps):
        wd = self.waited[e]
        for k, v in deps.items():
            if k == e and (e == "pe" or not self.same_engine_sync):
                continue
            if wd.get(k, 0) >= v:
                continue
            self.eng[e].wait_ge(self.sems[k], v)
            self.n_wait += 1
            wd[k] = v

    def _mark(self, ev, reads, writes):
        k, v = ev
        for r in reads:
            if r.r.get(k, 0) < v:
                r.r[k] = v
        for w in writes:
            w.w = ev
            w.r = {}

    def op(self, e, fn, reads=(), writes=()):
        self._wait(e, self._deps(reads, writes, eng=e))
        inst = fn(self.eng[e])
        self.cnt[e] += 1
        inst.then_inc(self.sems[e], 1)
        self._mark((e, self.cnt[e]), reads, writes)
        self.n_inst += 1
        return inst

    def _dma_ev(self, q, inst_fn):
        kind = "sw" if q == "pool" else "hw"
        pool = self.dsems[kind]
        k = pool[self.dsem_i[kind] % len(pool)]
        self.dsem_i[kind] += 1
        if self.cnt[k]:
            self._wait(q, {k: self.cnt[k]})
        inst = inst_fn()
        self.cnt[k] += 16
        inst.then_inc(self.sems[k], 16)
        self.n_inst += 1
        return (k, self.cnt[k])

    def dma(self, q, out, in_, reads, writes, sres=None, **kw):
        self._wait(q, self._deps(reads, writes))
        ev = self._dma_ev(q, lambda: self.eng[q].dma_start(out=out, in_=in_, **kw))
        self._mark(ev, reads, writes)

    def idma(self, out, out_offset, in_, in_offset, reads, writes, sres=None, **kw):
        self._wait("pool", self._deps(reads, writes))
        ev = self._dma_ev("pool", lambda: self.nc.gpsimd.indirect_dma_start(
            out=out, out_offset=out_offset, in_=in_, in_offset=in_offset, **kw))
        self._mark(ev, reads, writes)

    def barrier(self, release=True):
        evs = {}
        for e in self.eng:
            if self.cnt[e]:
                evs[e] = self.cnt[e]
        for k in self.sems:
            if k not in self.eng and self.cnt[k]:
                evs[k] = self.cnt[k]
        for e in self.eng:
            d = dict(evs)
            d.pop(e, None) if e == "pe" else None
            self._wait(e, d)

    def finish(self):
        self.barrier(release=False)
        self.stack.close()


class T:
    def __init__(self, h, name):
        self.h = h
        self.r = Res(name)

    def __getitem__(self, idx):
        return self.h[idx]


def build_nc(stop="all"):
    nc = bass.Bass("TRN2", target_bir_lowering=False)

    def din(name, shape, dt=F32):
        return nc.dram_tensor(name, list(shape), dt, kind="ExternalInput").ap()

    def dscr(name, shape, dt):
        return nc.dram_tensor(name, list(shape), dt, kind="Internal").ap()

    x_in = din("x", [NS, SEQ, D])
    cvec = din("cvec", [4, D])
    ctx_in = din("ctx", [NS, CTX, D])
    w_mod = din("w_mod", [2, D, 6 * D])
    b_mod = din("b_mod", [2, 6 * D])
    norm1 = din("norm1", [2, D])
    norm2 = din("norm2", [2, D])
    even_w_in = din("even_w_in", [D, 3072])
    even_lambda = din("even_lambda", [4, 64])
    even_subln = din("even_subln", [128])
    even_conv_w = din("even_conv_w", [3, 512])
    odd_w_in = din("odd_w_in", [D, 2048])
    odd_v_norm = din("odd_v_norm", [D])
    odd_w_s = din("odd_w_s", [4, 128, 128])
    odd_b_s = din("odd_b_s", [4, 128])
    w_out = din("w_out", [2, D, D])
    w_router = din("w_router", [2, D, NE])
    w_gate = din("w_gate", [2, NE, D, D])
    w_up = din("w_up", [2, NE, D, D])
    w_down = din("w_down", [2, NE, D, D])
    final_norm = din("final_norm", [D])
    rope_cos = din("rope_cos", [128, SEQ])
    rope_sin = din("rope_sin", [128, SEQ])
    out_d = nc.dram_tensor("out", [NS, SEQ, D], F32, kind="ExternalOutput").ap()
    if stop != "all":
        dbg_idx = nc.dram_tensor("dbg_idx", [128, 2, 64], I32, kind="ExternalOutput").ap()
        dbg_val = nc.dram_tensor("dbg_val", [128, 2, 64], F32, kind="ExternalOutput").ap()

    xd = [dscr("xd%d" % s_, [SEQ, D], F32) for s_ in range(NS)]
    h2d = [dscr("h2d%d" % s_, [SEQ, D], BF16) for s_ in range(NS)]
    qTd = dscr("qTd", [NS, 4, 128, SEQ], BF16)
    kTd = dscr("kTd", [NS, 4, 128, SEQ + CTX], BF16)
    vd = dscr("vd", [NS, SEQ + CTX, 512], BF16)
    ud = dscr("ud", [NS, 512, SEQ], F32)
    gbd = dscr("gbd", [NS, 512, SEQ], F32)
    mixTd = dscr("mixTd", [NS, D, SEQ], BF16)

    k = K(nc)
    dummy = Res("dummy")
    NKV = SEQ + CTX
    with contextlib.ExitStack() as gst, nc.allow_non_contiguous_dma(reason="small strided parameter loads"):
        cnt = [0]

        def alloc(st, shape, dt, name=None):
            cnt[0] += 1
            nm = "%s_%d" % (name or "t", cnt[0])
            return T(st.enter_context(nc.sbuf_tensor(nm, list(shape), dt)), nm)

        G = lambda shape, dt, name=None: alloc(gst, shape, dt, name)
        class V:
            def __init__(self, ap, name):
                self.ap = ap
                self.r = Res(name)

            def __getitem__(self, idx):
                return self.ap[idx]

        psd = [gst.enter_context(nc.psum_tensor("psd%d" % i, [128, 1024], F32)) for i in range(4)]
        ps = [V(psd[i // 2][:, (i % 2) * 512:(i % 2 + 1) * 512], "psb%d" % i) for i in range(8)]
        psi = [0]

        def nps():
            psi[0] += 1
            return ps[psi[0] % 8]

        def bank_pool(banks):
            c = [0]

            def f():
                c[0] += 1
                return ps[banks[c[0] % len(banks)]]
            return f

        def run_skewed(n, stages):
            S = len(stages)
            for t in range(n + S - 1):
                for j in reversed(range(S)):
                    i = t - j
                    if 0 <= i < n:
                        stages[j](i)

        identf = G([128, 128], F32, "identf")
        identb = G([128, 128], BF16, "identb")
        onesf = G([128, 128], F32, "onesf")
        k.op("pool", lambda e: e.memset(identf[:], 0.0), writes=[identf.r])
        k.op("pool", lambda e: e.affine_select(out=identf[:], in_=identf[:], pattern=[[-1, 128]],
                                               compare_op=ALU.not_equal, fill=1.0, base=0, channel_multiplier=1),
             reads=[identf.r], writes=[identf.r])
        k.op("dve", lambda e: e.tensor_copy(out=identb[:], in_=identf[:]), reads=[identf.r], writes=[identb.r])
        k.op("pool", lambda e: e.memset(onesf[:], 1.0), writes=[onesf.r])
        epsb = G([128, 1], F32, "epsb")
        k.op("pool", lambda e: e.memset(epsb[:], EPS), writes=[epsb.r])

        n1T = G([128, 2, 8], F32, "n1T")
        n2T = G([128, 2, 8], F32, "n2T")
        bmT = G([128, 2, 48], F32, "bmT")
        fnT = G([128, 8], F32, "fnT")
        vnT = G([128, 8], F32, "vnT")
        slT = G([128, 1], F32, "slT")
        cwT = G([128, 3, 4], F32, "cwT")
        bsT = G([128, 4], F32, "bsT")
        scT = G([128, 8, 4], F32, "scT")
        wr = [G([128, 8, NE], F32, "wr%d" % l) for l in range(2)]
        lamb = G([128, 4, 64], F32, "lamb")
        k.dma("sp", n1T[:], norm1.rearrange("l (j p) -> p l j", p=128), [], [n1T.r])
        k.dma("sp", n2T[:], norm2.rearrange("l (j p) -> p l j", p=128), [], [n2T.r])
        k.dma("sp", bmT[:], b_mod.rearrange("l (j p) -> p l j", p=128), [], [bmT.r])
        k.dma("sp", fnT[:], final_norm.rearrange("(j p) -> p j", p=128), [], [fnT.r])
        k.dma("sp", vnT[:], odd_v_norm.rearrange("(j p) -> p j", p=128), [], [vnT.r])
        k.dma("sp", slT[:], even_subln.rearrange("(j p) -> p j", p=128), [], [slT.r])
        k.dma("sp", cwT[:], even_conv_w.rearrange("i (c p) -> p i c", p=128), [], [cwT.r])
        k.dma("sp", bsT[:], odd_b_s.rearrange("g i -> i g"), [], [bsT.r])
        for r in range(4):
            k.dma("sp", scT[:, :, r], cvec[r].rearrange("(j p) -> p j", p=128), [scT.r], [scT.r])
        for l in range(2):
            k.dma("sp", wr[l][:], w_router[l].rearrange("(j p) e -> p j e", p=128), [], [wr[l].r])
        k.dma("sp", lamb[:], even_lambda.partition_broadcast(128), [], [lamb.r])
        k.op("act", lambda e: e.activation(out=scT[:], in_=scT[:], func=AF.Silu), reads=[scT.r], writes=[scT.r])

        LAM_INIT = 0.8 - 0.6 * math.exp(-0.3 * 0)
        lprod = G([128, 2, 64], F32, "lprod")
        lsum = G([128, 2], F32, "lsum")
        lamn = G([128, 1], F32, "lamn")
        k.op("dve", lambda e: e.tensor_tensor(out=lprod[:, 0, :], in0=lamb[:, 0, :], in1=lamb[:, 1, :], op=ALU.mult),
             reads=[lamb.r], writes=[lprod.r])
        k.op("dve", lambda e: e.tensor_tensor(out=lprod[:, 1, :], in0=lamb[:, 2, :], in1=lamb[:, 3, :], op=ALU.mult),
             reads=[lamb.r, lprod.r], writes=[lprod.r])
        k.op("dve", lambda e: e.tensor_reduce(out=lsum[:], in_=lprod[:], axis=AX.X, op=ALU.add),
             reads=[lprod.r], writes=[lsum.r])
        k.op("act", lambda e: e.activation(out=lsum[:], in_=lsum[:], func=AF.Exp), reads=[lsum.r], writes=[lsum.r])
        k.op("dve", lambda e: e.tensor_tensor(out=lamn[:], in0=lsum[:, 1:2], in1=lsum[:, 0:1], op=ALU.subtract),
             reads=[lsum.r], writes=[lamn.r])
        k.op("dve", lambda e: e.tensor_scalar(out=lamn[:], in0=lamn[:], scalar1=-LAM_INIT, scalar2=None, op0=ALU.add),
             reads=[lamn.r], writes=[lamn.r])

        modT = [G([128, 48, 4], F32, "modT%d" % l) for l in range(2)]
        a1T = [G([128, 8, 4], F32, "a1T%d" % l) for l in range(2)]
        a2T = [G([128, 8, 4], F32, "a2T%d" % l) for l in range(2)]
        with contextlib.ExitStack() as st:
            wmb = [alloc(st, [128, 8, 512], F32, "wmb") for _ in range(3)]
            modrow = alloc(st, [4, 6 * D], F32, "modrow")
            brow = alloc(st, [4, 6 * D], F32, "brow")
            for l in range(2):
                k.dma("sp", brow[:], b_mod[l].partition_broadcast(4), [modrow.r], [brow.r])
                for jb in range(12):
                    wm = wmb[(l * 12 + jb) % 3]
                    k.dma("sp", wm[:], w_mod[l][:, jb * 512:(jb + 1) * 512].rearrange("(j p) n -> p j n", p=128),
                          [], [wm.r])
                    pb = nps()

                    def mm(e, wm=wm, pb=pb):
                        for kc in range(8):
                            i = e.matmul(pb[0:4, :], lhsT=scT[:, kc, :], rhs=wm[:, kc, :], start=(kc == 0), stop=(kc == 7))
                        return i
                    k.op("pe", mm, reads=[wm.r, scT.r], writes=[pb.r])
                    k.op("dve", lambda e, jb=jb, pb=pb: e.tensor_tensor(
                        out=modrow[:, jb * 512:(jb + 1) * 512], in0=pb[0:4, :], in1=brow[:, jb * 512:(jb + 1) * 512],
                        op=ALU.add), reads=[pb.r, brow.r], writes=[modrow.r])
                pm = nps()

                def trs(e, pm=pm):
                    for j in range(48):
                        i = e.transpose(out=pm[:, j * 4:(j + 1) * 4], in_=modrow[0:4, j * 128:(j + 1) * 128],
                                        identity=identf[0:4, 0:4])
                    return i
                k.op("pe", trs, reads=[modrow.r, identf.r], writes=[pm.r])
                k.op("dve", lambda e, pm=pm, l=l: e.tensor_copy(
                    out=modT[l][:].rearrange("p j r -> p (j r)"), in_=pm[:, 0:192]), reads=[pm.r], writes=[modT[l].r])
                for r in range(3):
                    k.op("dve", lambda e, r=r, l=l: e.scalar_tensor_tensor(
                        out=a1T[l][:, :, r], in0=modT[l][:, 8:16, r], scalar=1.0, in1=n1T[:, l, :],
                        op0=ALU.add, op1=ALU.mult), reads=[modT[l].r, n1T.r, a1T[l].r], writes=[a1T[l].r])
                    k.op("dve", lambda e, r=r, l=l: e.scalar_tensor_tensor(
                        out=a2T[l][:, :, r], in0=modT[l][:, 32:40, r], scalar=1.0, in1=n2T[:, l, :],
                        op0=ALU.add, op1=ALU.mult), reads=[modT[l].r, n2T.r, a2T[l].r], writes=[a2T[l].r])
            k.barrier()

        diag = [G([128, 128], F32, "diag") for _ in range(2)]
        dgi = [0]

        def bcast(dst, col_fn, n, srcres):
            for q in range((n + 3) // 4):
                pb = nps()
                for jj in range(min(4, n - q * 4)):
                    j = q * 4 + jj
                    dg = diag[dgi[0] % 2]
                    dgi[0] += 1
                    k.op("dve", lambda e, dg=dg, j=j: e.tensor_scalar(out=dg[:], in0=identf[:], scalar1=col_fn(j),
                                                                     scalar2=None, op0=ALU.mult),
                         reads=[identf.r, srcres], writes=[dg.r])
                    k.op("pe", lambda e, dg=dg, jj=jj, pb=pb: e.matmul(pb[:, jj * 128:(jj + 1) * 128], lhsT=onesf[:],
                                                                        rhs=dg[:], start=True, stop=True),
                         reads=[onesf.r, dg.r], writes=[pb.r])
                w = min(4, n - q * 4) * 128
                k.op("act", lambda e, pb=pb, q=q, w=w: e.copy(out=dst[:, q * 512:q * 512 + w], in_=pb[:, 0:w]),
                     reads=[pb.r], writes=[dst.r])

        def rstd_from_ss(ss, rs, n, scale):
            k.op("act", lambda e: e.activation(out=rs[:, 0:n], in_=ss[:, 0:n], func=AF.Ln, bias=epsb[:, 0:1], scale=scale),
                 reads=[ss.r, epsb.r], writes=[rs.r])
            k.op("act", lambda e: e.activation(out=rs[:, 0:n], in_=rs[:, 0:n], func=AF.Exp, scale=-0.5),
                 reads=[rs.r], writes=[rs.r])

        probsT = G([64, SEQ], F32, "probsT")
        tvals = G([64, CAP], F32, "tvals")
        tidx = G([64, CAP], U32, "tidx")
        tidxf = G([64, CAP], F32, "tidxf")
        idxT = G([128, 2, 64], I32, "idxT")
        valT = G([128, 2, 64], F32, "valT")
        k.op("pool", lambda e: e.memset(probsT[:], 0.0), writes=[probsT.r])

        def phase_l0_inproj():
            with contextlib.ExitStack() as st:
                A = lambda shape, dt, name=None: alloc(st, shape, dt, name)
                win = A([128, 8, 3072], BF16, "win")
                wrot = A([128, 8, 1024], BF16, "wrot")
                cosT = A([128, SEQ], F32, "cosT")
                sinT = A([128, SEQ], F32, "sinT")
                for cb in range(3):
                    k.dma("pool", win[:, :, cb * 1024:(cb + 1) * 1024],
                          even_w_in[:, cb * 1024:(cb + 1) * 1024].rearrange("(j p) n -> p j n", p=128), [], [win.r])
                k.dma("sp", cosT[:], rope_cos, [], [cosT.r])
                k.dma("sp", sinT[:], rope_sin, [], [sinT.r])
                for kc in range(8):
                    src = win[:, kc, 0:1024].rearrange("p (g h c) -> p g h c", h=2, c=16)
                    dst = wrot[:, kc, :].rearrange("p (g h c) -> p g h c", h=2, c=16)
                    k.op("act", lambda e, src=src, dst=dst: e.mul(dst[:, :, 0, :], src[:, :, 1, :], -1.0),
                         reads=[win.r], writes=[wrot.r])
                    k.op("dve", lambda e, src=src, dst=dst: e.tensor_copy(out=dst[:, :, 1, :], in_=src[:, :, 0, :]),
                         reads=[win.r], writes=[wrot.r])
                xts = [A([128, 4, D], F32, "xt") for _ in range(2)]
                hTs = [A([128, 8, 512], BF16, "hT") for _ in range(2)]
                junk = A([128, D], BF16, "junk")
                ss = A([128, 4], F32, "ss")
                rs = A([128, 4], F32, "rs")
                qst = A([128, 4, 512], BF16, "qst")
                kst = A([128, 4, 512], BF16, "kst")
                ust = A([128, 4, 512], F32, "ust")
                gbst = A([128, 4, 512], F32, "gbst")
                vst = A([128, 4, 512], BF16, "vst")
                gcsb = [A([128, 512], F32, "gcsb") for _ in range(2)]
                t1s = [A([128, 512], F32, "t1") for _ in range(2)]
                t2s = [A([128, 512], F32, "t2") for _ in range(2)]
                work = []
                for s in range(NS):
                    work.append((s, "ctx", 0))
                    for tb in range(4):
                        work.append((s, "lat", tb))

                def load(i):
                    s, kind, tb = work[i]
                    xt = xts[i % 2]
                    if kind == "ctx":
                        k.dma("sp", xt[:, 0:2, :], ctx_in[s].rearrange("(c p) d -> p c d", p=128), [], [xt.r])
                    else:
                        k.dma("sp", xt[:], x_in[s][tb * 512:(tb + 1) * 512, :].rearrange("(c p) d -> p c d", p=128),
                              [], [xt.r])

                ri = [0]

                def meta(i):
                    s, kind, tb = work[i]
                    nch = 2 if kind == "ctx" else 4
                    return s, kind, tb, nch, nch * 128, (2 if kind == "ctx" else s)

                pA = bank_pool([0, 1])
                pB = bank_pool([2, 3, 4])
                pC = bank_pool([5, 6, 7])

                def s_norm(i):
                    s, kind, tb, nch, ntok, r = meta(i)
                    xt, hT = xts[i % 2], hTs[i % 2]
                    for c in range(nch):
                        k.op("act", lambda e, c=c: e.activation(out=junk[:], in_=xt[:, c, :], func=AF.Square,
                                                                accum_out=ss[:, c:c + 1]),
                             reads=[xt.r], writes=[junk.r, ss.r])
                    rstd_from_ss(ss, rs, nch, 1.0 / D)
                    for c in range(nch):
                        k.op("act", lambda e, c=c: e.activation(out=xt[:, c, :], in_=xt[:, c, :], func=AF.Copy,
                                                                scale=rs[:, c:c + 1]),
                             reads=[xt.r, rs.r], writes=[xt.r])
                    for kc in range(8):
                        pb = pA()

                        def tr(e, kc=kc, pb=pb):
                            for c in range(nch):
                                ii = e.transpose(out=pb[:, c * 128:(c + 1) * 128], in_=xt[:, c, kc * 128:(kc + 1) * 128],
                                                 identity=identf[:])
                            return ii
                        k.op("pe", tr, reads=[xt.r, identf.r], writes=[pb.r])
                        k.op("dve", lambda e, kc=kc, pb=pb: e.tensor_scalar(
                            out=hT[:, kc, 0:ntok], in0=pb[:, 0:ntok], scalar1=a1T[0][:, kc, r:r + 1],
                            scalar2=modT[0][:, kc, r:r + 1], op0=ALU.mult, op1=ALU.add),
                            reads=[pb.r, a1T[0].r, modT[0].r], writes=[hT.r])

                def proj(wt, c0, pb, hT, ntok):
                    def mm(e):
                        for kc in range(8):
                            ii = e.matmul(pb[:, 0:ntok], lhsT=wt[:, kc, c0:c0 + 128], rhs=hT[:, kc, 0:ntok],
                                          start=(kc == 0), stop=(kc == 7))
                        return ii
                    k.op("pe", mm, reads=[wt.r, hT.r], writes=[pb.r])

                def s_qk(i):
                    s, kind, tb, nch, ntok, r = meta(i)
                    hT = hTs[i % 2]
                    for which in (["k"] if kind == "ctx" else ["q", "k"]):
                        stg = qst if which == "q" else kst
                        for h in range(4):
                            c0 = (0 if which == "q" else 512) + h * 128
                            pa = pB()
                            proj(win, c0, pa, hT, ntok)
                            if kind == "ctx":
                                k.op("act", lambda e, pa=pa, h=h, stg=stg: e.copy(out=stg[:, h, 0:ntok], in_=pa[:, 0:ntok]),
                                     reads=[pa.r], writes=[stg.r])
                                continue
                            pr_ = pB()
                            proj(wrot, c0, pr_, hT, ntok)
                            t1 = t1s[ri[0] % 2]
                            t2 = t2s[ri[0] % 2]
                            ri[0] += 1
                            k.op("dve", lambda e, pa=pa, t1=t1: e.tensor_tensor(
                                out=t1[:], in0=pa[:], in1=cosT[:, tb * 512:(tb + 1) * 512], op=ALU.mult),
                                reads=[pa.r, cosT.r], writes=[t1.r])
                            k.op("dve", lambda e, pr_=pr_, t2=t2: e.tensor_tensor(
                                out=t2[:], in0=pr_[:], in1=sinT[:, tb * 512:(tb + 1) * 512], op=ALU.mult),
                                reads=[pr_.r, sinT.r], writes=[t2.r])
                            k.op("dve", lambda e, t1=t1, t2=t2, h=h, stg=stg: e.tensor_tensor(
                                out=stg[:, h, :], in0=t1[:], in1=t2[:], op=ALU.add),
                                reads=[t1.r, t2.r], writes=[stg.r])
                        if which == "q":
                            k.dma("sp", qTd[s].rearrange("h p t -> p h t")[:, :, tb * 512:(tb + 1) * 512], qst[:],
                                  [qst.r], [dummy])
                        else:
                            t0 = 0 if kind == "ctx" else CTX + tb * 512
                            k.dma("sp", kTd[s].rearrange("h p t -> p h t")[:, :, t0:t0 + ntok], kst[:, :, 0:ntok],
                                  [kst.r], [dummy])

                def s_vconv(i):
                    s, kind, tb, nch, ntok, r = meta(i)
                    hT = hTs[i % 2]
                    for c in range(nch):
                        pv = pC()

                        def mmv(e, c=c, pv=pv):
                            for kc in range(8):
                                ii = e.matmul(pv[:], lhsT=hT[:, kc, c * 128:(c + 1) * 128], rhs=win[:, kc, 1024:1536],
                                              start=(kc == 0), stop=(kc == 7))
                            return ii
                        k.op("pe", mmv, reads=[win.r, hT.r], writes=[pv.r])
                        k.op("act", lambda e, c=c, pv=pv: e.copy(out=vst[:, c, :], in_=pv[:]), reads=[pv.r], writes=[vst.r])
                    t0 = 0 if kind == "ctx" else CTX + tb * 512
                    k.dma("sp", vd[s][t0:t0 + ntok, :].rearrange("(c p) v -> p c v", p=128), vst[:, 0:nch, :],
                          [vst.r], [dummy])
                    if kind == "ctx":
                        return
                    for cc in range(4):
                        pgb = pC()
                        proj(win, 1536 + cc * 128, pgb, hT, ntok)
                        k.op("act", lambda e, cc=cc, pgb=pgb: e.copy(out=gbst[:, cc, :], in_=pgb[:]),
                             reads=[pgb.r], writes=[gbst.r])
                        pgc = pC()
                        proj(win, 2048 + cc * 128, pgc, hT, ntok)
                        gc_ = gcsb[cc % 2]
                        k.op("act", lambda e, pgc=pgc, gc_=gc_: e.copy(out=gc_[:], in_=pgc[:]), reads=[pgc.r], writes=[gc_.r])
                        pxs = pC()
                        proj(win, 2560 + cc * 128, pxs, hT, ntok)
                        k.op("dve", lambda e, cc=cc, pxs=pxs, gc_=gc_: e.tensor_tensor(
                            out=ust[:, cc, :], in0=pxs[:], in1=gc_[:], op=ALU.mult),
                            reads=[pxs.r, gc_.r], writes=[ust.r])
                    k.dma("sp", gbd[s].rearrange("(c p) t -> p c t", p=128)[:, :, tb * 512:(tb + 1) * 512], gbst[:],
                          [gbst.r], [dummy])
                    k.dma("sp", ud[s].rearrange("(c p) t -> p c t", p=128)[:, :, tb * 512:(tb + 1) * 512], ust[:],
                          [ust.r], [dummy])

                run_skewed(len(work), [load, s_norm, s_qk, s_vconv])
                k.barrier()

        def phase_l0_conv():
            with contextlib.ExitStack() as st:
                A = lambda shape, dt, name=None: alloc(st, shape, dt, name)
                ups = [A([128, SEQ + 2], F32, "upad") for _ in range(2)]
                gbs = [A([128, SEQ], F32, "gbs") for _ in range(2)]
                acc = [A([128, SEQ], F32, "cacc") for _ in range(2)]
                mst = [A([128, SEQ], BF16, "cmix") for _ in range(2)]
                for u_ in ups:
                    k.op("pool", lambda e, u_=u_: e.memset(u_[:, 0:1], 0.0), writes=[u_.r])
                    k.op("pool", lambda e, u_=u_: e.memset(u_[:, SEQ + 1:SEQ + 2], 0.0), reads=[u_.r], writes=[u_.r])
                i = 0
                for s in range(NS):
                    for cc in range(4):
                        up, gb, ac, ms = ups[i % 2], gbs[i % 2], acc[i % 2], mst[i % 2]
                        i += 1
                        k.dma("sp", up[:, 1:SEQ + 1], ud[s][cc * 128:(cc + 1) * 128, :], [], [up.r])
                        k.dma("sp", gb[:], gbd[s][cc * 128:(cc + 1) * 128, :], [], [gb.r])
                        k.op("act", lambda e, up=up, ac=ac, cc=cc: e.activation(
                            out=ac[:], in_=up[:, 0:SEQ], func=AF.Copy, scale=cwT[:, 0, cc:cc + 1]),
                            reads=[up.r, cwT.r], writes=[ac.r])
                        for tap in (1, 2):
                            k.op("dve", lambda e, up=up, ac=ac, cc=cc, tap=tap: e.scalar_tensor_tensor(
                                out=ac[:], in0=up[:, tap:tap + SEQ], scalar=cwT[:, tap, cc:cc + 1], in1=ac[:],
                                op0=ALU.mult, op1=ALU.add), reads=[up.r, cwT.r, ac.r], writes=[ac.r])
                        k.op("dve", lambda e, ac=ac, gb=gb, ms=ms: e.tensor_tensor(out=ms[:], in0=ac[:], in1=gb[:],
                                                                                    op=ALU.mult),
                             reads=[ac.r, gb.r], writes=[ms.r])
                        k.dma("sp", mixTd[s][512 + cc * 128:512 + (cc + 1) * 128, :], ms[:], [ms.r], [dummy])
                k.barrier()

        def phase_l0_attn():
            with contextlib.ExitStack() as st:
                A = lambda shape, dt, name=None: alloc(st, shape, dt, name)
                NKC = NKV // 128
                qTs = [A([128, SEQ], BF16, "qT") for _ in range(2)]
                kTs = [A([128, NKV], BF16, "kT") for _ in range(2)]
                vas = [A([128, NKC, 130], BF16, "vaug") for _ in range(2)]
                PTs = [A([128, NKC, 1024], BF16, "PT") for _ in range(2)]
                osb = [[A([128, 4, 130], F32, "osb") for _ in range(2)] for _ in range(3)]
                mixst = [A([128, SEQ], BF16, "mixst") for _ in range(2)]
                sublnb = A([128, 128], F32, "sublnb")
                slsc = A([128, 1], F32, "slsc")
                rec = [A([128, 2, 4], F32, "rec") for _ in range(3)]
                ass = [A([128, 4], F32, "ass") for _ in range(3)]
                ars = [A([128, 4], F32, "ars") for _ in range(3)]
                tt = [A([128, 128], F32, "att") for _ in range(2)]
                aa = [A([128, 4, 128], F32, "aa") for _ in range(3)]
                ajunk = A([128, 128], BF16, "ajunk")
                ajunkf = A([128, 128], F32, "ajunkf")
                attb = [A([128, 128], BF16, "attb") for _ in range(2)]
                k.op("dve", lambda e: e.tensor_scalar(out=slsc[:], in0=slT[:], scalar1=1.0 - LAM_INIT, scalar2=None,
                                                      op0=ALU.mult), reads=[slT.r], writes=[slsc.r])
                bcast(sublnb, lambda j: slsc[:, 0:1], 1, slsc.r)
                for va in vas:
                    k.op("pool", lambda e, va=va: e.memset(va[:, :, 128:129], 1.0), writes=[va.r])
                    k.op("pool", lambda e, va=va: e.memset(va[:, :, 129:130], 0.0), reads=[va.r], writes=[va.r])
                ups = [A([128, SEQ + 2], F32, "upad") for _ in range(2)]
                gbs = [A([128, SEQ], F32, "gbs") for _ in range(2)]
                cacc = [A([128, SEQ], F32, "cacc") for _ in range(2)]
                cmst = [A([128, SEQ], BF16, "cmix") for _ in range(2)]
                for u_ in ups:
                    k.op("pool", lambda e, u_=u_: e.memset(u_[:, 0:1], 0.0), writes=[u_.r])
                    k.op("pool", lambda e, u_=u_: e.memset(u_[:, SEQ + 1:SEQ + 2], 0.0), reads=[u_.r], writes=[u_.r])

                def conv_item(ci):
                    s, cc = ci // 4, ci % 4
                    up, gb, ac, ms = ups[ci % 2], gbs[ci % 2], cacc[ci % 2], cmst[ci % 2]
                    k.dma("sp", up[:, 1:SEQ + 1], ud[s][cc * 128:(cc + 1) * 128, :], [], [up.r])
                    k.dma("sp", gb[:], gbd[s][cc * 128:(cc + 1) * 128, :], [], [gb.r])
                    k.op("act", lambda e: e.activation(out=ac[:], in_=up[:, 0:SEQ], func=AF.Copy, scale=cwT[:, 0, cc:cc + 1]),
                         reads=[up.r, cwT.r], writes=[ac.r])
                    for tap in (1, 2):
                        k.op("dve", lambda e, tap=tap: e.scalar_tensor_tensor(
                            out=ac[:], in0=up[:, tap:tap + SEQ], scalar=cwT[:, tap, cc:cc + 1], in1=ac[:],
                            op0=ALU.mult, op1=ALU.add), reads=[up.r, cwT.r, ac.r], writes=[ac.r])
                    k.op("dve", lambda e: e.tensor_tensor(out=ms[:], in0=ac[:], in1=gb[:], op=ALU.mult),
                         reads=[ac.r, gb.r], writes=[ms.r])
                    k.dma("sp", mixTd[s][512 + cc * 128:512 + (cc + 1) * 128, :], ms[:], [ms.r], [dummy])

                heads = [(s, h) for s in range(NS) for h in range(4)]
                steps = [(hi, Q) for hi in range(len(heads)) for Q in range(4)]
                NST = len(steps)
                sbank = [0]
                abank = [0]

                def load_head(hi):
                    s, h = heads[hi]
                    k.dma("sp", qTs[hi % 2][:], qTd[s][h], [], [qTs[hi % 2].r])
                    k.dma("sp", kTs[hi % 2][:], kTd[s][h], [], [kTs[hi % 2].r])
                    k.dma("sp", vas[hi % 2][:, :, 0:128],
                          vd[s][:, h * 128:(h + 1) * 128].rearrange("(c p) v -> p c v", p=128), [], [vas[hi % 2].r])

                def units_S(si):
                    hi, Q = steps[si]
                    qT, kT, PT = qTs[hi % 2], kTs[hi % 2], PTs[si % 2]
                    out = []
                    for c in range(NKC):
                        def u(c=c):
                            if c == 0 and Q == 0:
                                load_head(hi)
                            d = sbank[0] % 3
                            sbank[0] += 1
                            b0, b1 = ps[2 * d], ps[2 * d + 1]

                            def mm(e):
                                e.matmul(b0[:], lhsT=kT[0:64, c * 128:(c + 1) * 128], rhs=qT[0:64, Q * 512:(Q + 1) * 512],
                                         start=True, stop=True)
                                return e.matmul(b1[:], lhsT=kT[64:128, c * 128:(c + 1) * 128],
                                                rhs=qT[64:128, Q * 512:(Q + 1) * 512], start=True, stop=True)
                            k.op("pe", mm, reads=[kT.r, qT.r], writes=[b0.r, b1.r])
                            k.op("act", lambda e: e.activation(out=PT[:, c, :], in_=psd[d][:], func=AF.Exp, scale=0.125),
                                 reads=[b0.r, b1.r], writes=[PT.r])
                        out.append(u)
                    return out

                def units_AV(si):
                    hi, Q = steps[si]
                    PT, va = PTs[si % 2], vas[hi % 2]
                    out = []
                    for m in range(2):
                        ob = osb[si % 3][m]
                        for j in range(4):
                            def u(m=m, j=j, ob=ob):
                                pb = ps[6 + abank[0] % 2]
                                abank[0] += 1

                                def mm(e):
                                    for c in range(NKC):
                                        ii = e.matmul(pb[:, 0:130], lhsT=PT[:, c, m * 512 + j * 128:m * 512 + (j + 1) * 128],
                                                      rhs=va[:, c, :], start=(c == 0), stop=(c == NKC - 1))
                                    return ii
                                k.op("pe", mm, reads=[PT.r, va.r], writes=[pb.r])
                                k.op("dve", lambda e: e.tensor_copy(out=ob[:, j, :], in_=pb[:, 0:130]),
                                     reads=[pb.r], writes=[ob.r])
                            out.append(u)
                    return out

                def st_C1(si):
                    o1, o2 = osb[si % 3][0], osb[si % 3][1]
                    rc, as_, a_ = rec[si % 3], ass[si % 3], aa[si % 3]
                    k.op("dve", lambda e: e.reciprocal(out=rc[:, 0, :], in_=o1[:, :, 128]), reads=[o1.r], writes=[rc.r])
                    k.op("dve", lambda e: e.reciprocal(out=rc[:, 1, :], in_=o2[:, :, 128]), reads=[o2.r, rc.r], writes=[rc.r])
                    k.op("dve", lambda e: e.tensor_scalar(out=rc[:, 1, :], in0=rc[:, 1, :], scalar1=lamn[:, 0:1],
                                                          scalar2=None, op0=ALU.mult), reads=[rc.r, lamn.r], writes=[rc.r])
                    for j in range(4):
                        t_ = tt[j % 2]
                        k.op("dve", lambda e, j=j, t_=t_: e.tensor_scalar(out=t_[:], in0=o2[:, j, 0:128],
                                                                           scalar1=rc[:, 1, j:j + 1], scalar2=None,
                                                                           op0=ALU.mult),
                             reads=[o2.r, rc.r], writes=[t_.r])
                        k.op("dve", lambda e, j=j, t_=t_: e.scalar_tensor_tensor(
                            out=a_[:, j, :], in0=o1[:, j, 0:128], scalar=rc[:, 0, j:j + 1], in1=t_[:],
                            op0=ALU.mult, op1=ALU.add), reads=[o1.r, rc.r, t_.r], writes=[a_.r])
                        k.op("dve", lambda e, j=j: e.scalar_tensor_tensor(
                            out=ajunkf[:], in0=a_[:, j, :], scalar=1.0, in1=a_[:, j, :], op0=ALU.mult, op1=ALU.mult,
                            accum_out=as_[:, j:j + 1]), reads=[a_.r], writes=[ajunkf.r, as_.r])

                def st_C2(si):
                    rstd_from_ss(ass[si % 3], ars[si % 3], 4, 1.0 / 128)

                def st_C3(si):
                    hi, Q = steps[si]
                    s, h = heads[hi]
                    a_, ar = aa[si % 3], ars[si % 3]
                    ms = mixst[hi % 2]
                    for j in range(4):
                        ab = attb[j % 2]
                        k.op("dve", lambda e, j=j, ab=ab: e.scalar_tensor_tensor(
                            out=ab[:], in0=a_[:, j, :], scalar=ar[:, j:j + 1], in1=sublnb[:], op0=ALU.mult,
                            op1=ALU.mult), reads=[a_.r, ar.r, sublnb.r], writes=[ab.r])
                        pb = ps[6 + abank[0] % 2]
                        abank[0] += 1
                        pbb = pb[:].bitcast(BF16)
                        k.op("pe", lambda e, ab=ab, pbb=pbb: e.transpose(out=pbb[:, 0:128], in_=ab[:], identity=identb[:]),
                             reads=[ab.r, identb.r], writes=[pb.r])
                        k.op("dve", lambda e, j=j, pbb=pbb: e.tensor_copy(
                            out=ms[:, Q * 512 + j * 128:Q * 512 + (j + 1) * 128], in_=pbb[:, 0:128]),
                            reads=[pb.r], writes=[ms.r])
                    if Q == 3:
                        k.dma("sp", mixTd[s][h * 128:(h + 1) * 128, :], ms[:], [ms.r], [dummy])

                for t in range(NST + 4):
                    for fn, off in ((st_C3, 4), (st_C2, 3), (st_C1, 2)):
                        if 0 <= t - off < NST:
                            fn(t - off)
                    if t % 4 == 1 and t // 4 < 8:
                        conv_item(t // 4)
                    us_ = units_S(t) if t < NST else []
                    ua_ = units_AV(t - 1) if 0 <= t - 1 < NST else []
                    ai = 0
                    for ci, u in enumerate(us_):
                        u()
                        while ai < len(ua_) and (ai + 1) * len(us_) <= (ci + 1) * len(ua_) + len(ua_) - 1:
                            ua_[ai]()
                            ai += 1
                    while ai < len(ua_):
                        ua_[ai]()
                        ai += 1
                k.barrier()

        class TailBufs:
            pass

        def tail_setup(st, l):
            A = lambda shape, dt, name=None: alloc(st, shape, dt, name)
            tb = TailBufs()
            tb.wout = A([128, 8, D], BF16, "wout")
            k.dma("pool", tb.wout[:], w_out[l].rearrange("(j p) n -> p j n", p=128), [], [tb.wout.r])
            tb.g1b = [A([128, D], F32, "g1b") for _ in range(NS)]
            tb.a2b = [A([128, D], F32, "a2b") for _ in range(NS)]
            tb.sh2b = [A([128, D], F32, "sh2b") for _ in range(NS)]
            for s in range(NS):
                bcast(tb.g1b[s], lambda j, s=s: modT[l][:, 16 + j, s:s + 1], 8, modT[l].r)
                bcast(tb.a2b[s], lambda j, s=s: a2T[l][:, j, s:s + 1], 8, a2T[l].r)
                bcast(tb.sh2b[s], lambda j, s=s: modT[l][:, 24 + j, s:s + 1], 8, modT[l].r)
            tb.tmp = [A([128, D], F32, "ttmp") for _ in range(2)]
            tb.xnew = [A([128, D], F32, "xnew") for _ in range(3)]
            tb.h2f = [A([128, D], F32, "h2f") for _ in range(2)]
            tb.h2T = [A([128, 8, 128], F32, "h2T") for _ in range(2)]
            tb.junk = A([128, D], BF16, "tjunk")
            tb.ss = [A([128, 1], F32, "tss") for _ in range(4)]
            tb.rs = [A([128, 1], F32, "trs") for _ in range(4)]
            tb.mx = [A([128, 1], F32, "tmx") for _ in range(3)]
            tb.es = [A([128, 1], F32, "tes") for _ in range(3)]
            tb.ex = [A([128, NE], F32, "tex") for _ in range(3)]
            tb.pr2 = [A([128, 64], F32, "pr2") for _ in range(2)]
            for p_ in tb.pr2:
                k.op("pool", lambda e, p_=p_: e.memset(p_[:], 0.0), writes=[p_.r])
            return tb

        def tail_stages(tb, l, work, mix_of, xt_of, pools):
            R4, R3, R2 = 4, 3, 2

            def t1(i):
                ch, s = work[i]
                mix_lhs, mixres = mix_of(i)
                xt_ap, xtres = xt_of(i)
                tmp, xnew, ss = tb.tmp[i % 2], tb.xnew[i % R3], tb.ss[i % R4]
                for half in range(2):
                    pb = pools[0]()

                    def mm(e, half=half, pb=pb):
                        for kc in range(8):
                            ii = e.matmul(pb[:], lhsT=mix_lhs(kc), rhs=tb.wout[:, kc, half * 512:(half + 1) * 512],
                                          start=(kc == 0), stop=(kc == 7))
                        return ii
                    k.op("pe", mm, reads=[mixres, tb.wout.r], writes=[pb.r])
                    k.op("dve", lambda e, half=half, pb=pb: e.tensor_tensor(
                        out=tmp[:, half * 512:(half + 1) * 512], in0=pb[:], in1=tb.g1b[s][:, half * 512:(half + 1) * 512],
                        op=ALU.mult), reads=[pb.r, tb.g1b[s].r], writes=[tmp.r])
                k.op("dve", lambda e: e.tensor_tensor(out=xnew[:], in0=tmp[:], in1=xt_ap, op=ALU.add),
                     reads=[tmp.r, xtres], writes=[xnew.r])
                k.dma("sp", xd[s][ch * 128:(ch + 1) * 128, :], xnew[:], [xnew.r], [dummy])
                k.op("act", lambda e: e.activation(out=tb.junk[:], in_=xnew[:], func=AF.Square, accum_out=ss[:, 0:1]),
                     reads=[xnew.r], writes=[tb.junk.r, ss.r])

            def t2(i):
                rstd_from_ss(tb.ss[i % R4], tb.rs[i % R4], 1, 1.0 / D)

            def t3(i):
                ch, s = work[i]
                xnew, rs, h2f, h2T = tb.xnew[i % R3], tb.rs[i % R4], tb.h2f[i % R2], tb.h2T[i % R2]
                k.op("dve", lambda e: e.scalar_tensor_tensor(out=h2f[:], in0=xnew[:], scalar=rs[:, 0:1], in1=tb.a2b[s][:],
                                                             op0=ALU.mult, op1=ALU.mult),
                     reads=[xnew.r, rs.r, tb.a2b[s].r], writes=[h2f.r])
                k.op("dve", lambda e: e.tensor_tensor(out=h2f[:], in0=h2f[:], in1=tb.sh2b[s][:], op=ALU.add),
                     reads=[h2f.r, tb.sh2b[s].r], writes=[h2f.r])
                k.dma("pool", h2d[s][ch * 128:(ch + 1) * 128, :], h2f[:], [h2f.r], [dummy])
                for q in range(2):
                    pb = pools[1]()

                    def tr(e, q=q, pb=pb):
                        for c in range(4):
                            kc = q * 4 + c
                            ii = e.transpose(out=pb[:, c * 128:(c + 1) * 128], in_=h2f[:, kc * 128:(kc + 1) * 128],
                                             identity=identf[:])
                        return ii
                    k.op("pe", tr, reads=[h2f.r, identf.r], writes=[pb.r])
                    k.op("act", lambda e, q=q, pb=pb: e.copy(out=h2T[:, q * 4:(q + 1) * 4, :],
                                                             in_=pb[:].rearrange("p (c t) -> p c t", c=4)),
                         reads=[pb.r], writes=[h2T.r])

            def t4(i):
                h2T, mx, es, ex = tb.h2T[i % R2], tb.mx[i % R3], tb.es[i % R3], tb.ex[i % R3]
                pl = pools[2]()

                def mmr(e):
                    for kc in range(8):
                        ii = e.matmul(pl[:, 0:NE], lhsT=h2T[:, kc, :], rhs=wr[l][:, kc, :], start=(kc == 0), stop=(kc == 7))
                    return ii
                k.op("pe", mmr, reads=[h2T.r, wr[l].r], writes=[pl.r])
                k.op("dve", lambda e: e.tensor_reduce(out=mx[:], in_=pl[:, 0:NE], axis=AX.X, op=ALU.max, negate=True),
                     reads=[pl.r], writes=[mx.r])
                k.op("act", lambda e: e.activation(out=ex[:], in_=pl[:, 0:NE], func=AF.Exp, bias=mx[:, 0:1], scale=1.0,
                                                   accum_out=es[:, 0:1]), reads=[pl.r, mx.r], writes=[ex.r, es.r])

            def t5(i):
                ch, s = work[i]
                es, ex = tb.es[i % R3], tb.ex[i % R3]
                pr2 = tb.pr2[ch % 2]
                k.op("dve", lambda e: e.reciprocal(out=es[:], in_=es[:]), reads=[es.r], writes=[es.r])
                k.op("dve", lambda e: e.tensor_scalar(out=pr2[:, s * 32:s * 32 + NE], in0=ex[:], scalar1=es[:, 0:1],
                                                      scalar2=None, op0=ALU.mult), reads=[ex.r, es.r], writes=[pr2.r])
                if s == NS - 1:
                    pt = pools[3]()
                    k.op("pe", lambda e: e.transpose(out=pt[0:64, 0:128], in_=pr2[:], identity=identf[:]),
                         reads=[pr2.r, identf.r], writes=[pt.r])
                    k.op("act", lambda e: e.copy(out=probsT[:, ch * 128:(ch + 1) * 128], in_=pt[0:64, 0:128]),
                         reads=[pt.r], writes=[probsT.r])
            return [t1, t2, t3, t4, t5]

        def phase_l0_tail():
            with contextlib.ExitStack() as st:
                A = lambda shape, dt, name=None: alloc(st, shape, dt, name)
                tb = tail_setup(st, 0)
                mixTs = [A([128, 8, 128], BF16, "mixT") for _ in range(3)]
                xts = [A([128, D], F32, "xt") for _ in range(3)]
                work = [(ch, s) for ch in range(16) for s in range(NS)]

                def s_load(i):
                    ch, s = work[i]
                    k.dma("sp", mixTs[i % 3][:], mixTd[s].rearrange("(j p) t -> p j t", p=128)[:, :, ch * 128:(ch + 1) * 128],
                          [], [mixTs[i % 3].r])
                    k.dma("sp", xts[i % 3][:], x_in[s][ch * 128:(ch + 1) * 128, :], [], [xts[i % 3].r])

                stages = tail_stages(tb, 0, work,
                                     lambda i: ((lambda kc, mT=mixTs[i % 3]: mT[:, kc, :]), mixTs[i % 3].r),
                                     lambda i: (xts[i % 3][:], xts[i % 3].r),
                                     [bank_pool([0, 1, 2]), bank_pool([3, 4]), bank_pool([5, 6]), bank_pool([7])])
                run_skewed(len(work), [s_load] + stages)
                k.barrier()

        def phase_topk():
            for r in range(CAP // 8):
                sl = slice(r * 8, (r + 1) * 8)
                k.op("dve", lambda e, sl=sl: e.max(out=tvals[:, sl], in_=probsT[:]), reads=[probsT.r], writes=[tvals.r])
                k.op("dve", lambda e, sl=sl: e.max_index(out=tidx[:, sl], in_max=tvals[:, sl], in_values=probsT[:]),
                     reads=[probsT.r, tvals.r], writes=[tidx.r])
                k.op("dve", lambda e, sl=sl: e.match_replace(out=probsT[:], in_to_replace=tvals[:, sl],
                                                             in_values=probsT[:], imm_value=-1.0),
                     reads=[probsT.r, tvals.r], writes=[probsT.r])
            k.op("dve", lambda e: e.tensor_copy(out=tidxf[:], in_=tidx[:]), reads=[tidx.r], writes=[tidxf.r])
            for half in range(2):
                pa = nps()
                k.op("pe", lambda e, pa=pa, half=half: e.transpose(out=pa[:, 0:64], in_=tidxf[:, half * 128:(half + 1) * 128],
                                                                   identity=identf[0:64, 0:64]),
                     reads=[tidxf.r, identf.r], writes=[pa.r])
                k.op("dve", lambda e, pa=pa, half=half: e.tensor_copy(out=idxT[:, half, :], in_=pa[:, 0:64]),
                     reads=[pa.r], writes=[idxT.r])
                pv = nps()
                k.op("pe", lambda e, pv=pv, half=half: e.transpose(out=pv[:, 0:64], in_=tvals[:, half * 128:(half + 1) * 128],
                                                                   identity=identf[0:64, 0:64]),
                     reads=[tvals.r, identf.r], writes=[pv.r])
                k.op("act", lambda e, pv=pv, half=half: e.copy(out=valT[:, half, :], in_=pv[:, 0:64]),
                     reads=[pv.r], writes=[valT.r])
            k.op("pool", lambda e: e.memset(probsT[:], 0.0), reads=[probsT.r], writes=[probsT.r])
            k.barrier()

        def phase_moe(l):
            with contextlib.ExitStack() as st:
                A = lambda shape, dt, name=None: alloc(st, shape, dt, name)
                wg = [A([128, 8, D], BF16, "wg") for _ in range(2)]
                wu = [A([128, 8, D], BF16, "wu") for _ in range(2)]
                wd = [A([128, 8, D], BF16, "wd") for _ in range(2)]
                xs = [[A([128, D], BF16, "xs") for _ in range(4)] for _ in range(2)]
                xsT = [A([128, 8, 512], BF16, "xsT") for _ in range(2)]
                hT = [A([128, 8, 512], BF16, "hT") for _ in range(2)]
                sg = [A([128, 512], F32, "sg") for _ in range(2)]
                ysb = [A([128, D], F32, "ysb") for _ in range(4)]
                g2b = [A([128, D], F32, "g2b") for _ in range(NS)]
                for s in range(NS):
                    bcast(g2b[s], lambda j, s=s: modT[l][:, 40 + j, s:s + 1], 8, modT[l].r)
                rx = [Res("xd_s%d" % s) for s in range(NS)]

                def prefetch_w(e):
                    b = e % 2
                    for wt, src in ((wg[b], w_gate), (wu[b], w_up), (wd[b], w_down)):
                        k.dma("pool", wt[:], src[l][e].rearrange("(j p) n -> p j n", p=128), [], [wt.r])

                def prefetch_g(e):
                    b = e % 2
                    for s in range(NS):
                        for half in range(2):
                            t = xs[b][s * 2 + half]
                            k.idma(t[:], None, h2d[s], bass.IndirectOffsetOnAxis(
                                ap=idxT[:, half, s * 32 + e:s * 32 + e + 1], axis=0), [idxT.r], [t.r])

                def prefetch(e):
                    if e >= 2:
                        prefetch_w(e)
                    prefetch_g(e)

                prefetch_w(0)
                prefetch_w(1)
                phase_topk()
                import os
                MODE = os.environ.get("MK_MOE", "")
                NEX = 1 if MODE == "ne1" else NE
                prefetch_g(0)
                sgi = 0
                for e in range(NEX):
                    b = e % 2
                    if e + 1 < NEX:
                        prefetch(e + 1)
                    if MODE == "nocomp":
                        for tc in range(4):
                            s, half = tc // 2, tc % 2
                            y = ysb[tc]
                            k.op("dve", lambda e_, y=y, tc=tc: e_.tensor_copy(out=y[:], in_=xs[b][tc][:]),
                                 reads=[xs[b][tc].r, wg[b].r, wu[b].r, wd[b].r], writes=[y.r])
                            k.idma(xd[s], bass.IndirectOffsetOnAxis(ap=idxT[:, half, s * 32 + e:s * 32 + e + 1], axis=0),
                                   y[:], None, [y.r, idxT.r, rx[s]], [rx[s]], compute_op=ALU.add)
                        continue
                    for tc in range(4):
                        t = xs[b][tc]
                        pb = nps()
                        pbb = pb[:].bitcast(BF16)

                        def tr(e_, t=t, pbb=pbb):
                            for kc in range(8):
                                ii = e_.transpose(out=pbb[:, kc * 128:(kc + 1) * 128], in_=t[:, kc * 128:(kc + 1) * 128],
                                                  identity=identb[:])
                            return ii
                        k.op("pe", tr, reads=[t.r, identb.r], writes=[pb.r])
                        k.op("act", lambda e_, tc=tc, pbb=pbb: e_.copy(out=xsT[b][:, :, tc * 128:(tc + 1) * 128],
                                                                      in_=pbb.rearrange("p (j t) -> p j t", j=8)),
                             reads=[pb.r], writes=[xsT[b].r])
                    for fc in range(8):
                        pg, pu = nps(), nps()
                        for wt, pb in ((wg[b], pg), (wu[b], pu)):
                            def mm(e_, wt=wt, pb=pb, fc=fc):
                                for kc in range(8):
                                    ii = e_.matmul(pb[:], lhsT=wt[:, kc, fc * 128:(fc + 1) * 128], rhs=xsT[b][:, kc, :],
                                                   start=(kc == 0), stop=(kc == 7))
                                return ii
                            k.op("pe", mm, reads=[wt.r, xsT[b].r], writes=[pb.r])
                        sg_ = sg[sgi % 2]
                        sgi += 1
                        k.op("act", lambda e_, pg=pg, sg_=sg_: e_.activation(out=sg_[:], in_=pg[:], func=AF.Silu),
                             reads=[pg.r], writes=[sg_.r])
                        k.op("dve", lambda e_, pu=pu, sg_=sg_, fc=fc: e_.tensor_tensor(out=hT[b][:, fc, :], in0=pu[:],
                                                                                     in1=sg_[:], op=ALU.mult),
                             reads=[pu.r, sg_.r], writes=[hT[b].r])
                    for tc in range(4):
                        s, half = tc // 2, tc % 2
                        y = ysb[tc]
                        for dh in range(2):
                            pb = nps()

                            def mmd(e_, tc=tc, dh=dh, pb=pb):
                                for fc in range(8):
                                    ii = e_.matmul(pb[:], lhsT=hT[b][:, fc, tc * 128:(tc + 1) * 128],
                                                   rhs=wd[b][:, fc, dh * 512:(dh + 1) * 512], start=(fc == 0), stop=(fc == 7))
                                return ii
                            k.op("pe", mmd, reads=[hT[b].r, wd[b].r], writes=[pb.r])
                            k.op("dve", lambda e_, pb=pb, dh=dh, y=y, s=s, half=half: e_.scalar_tensor_tensor(
                                out=y[:, dh * 512:(dh + 1) * 512], in0=pb[:], scalar=valT[:, half, s * 32 + e:s * 32 + e + 1],
                                in1=g2b[s][:, dh * 512:(dh + 1) * 512], op0=ALU.mult, op1=ALU.mult),
                                reads=[pb.r, valT.r, g2b[s].r], writes=[y.r])
                        k.idma(xd[s], bass.IndirectOffsetOnAxis(ap=idxT[:, half, s * 32 + e:s * 32 + e + 1], axis=0),
                               y[:], None, [y.r, idxT.r, rx[s]], [rx[s]], compute_op=ALU.add)
                k.barrier()

        def phase_l1():
            with contextlib.ExitStack() as st:
                A = lambda shape, dt, name=None: alloc(st, shape, dt, name)
                tb = tail_setup(st, 1)
                w1 = A([128, 8, 2048], BF16, "w1in")
                for cb in range(2):
                    k.dma("pool", w1[:, :, cb * 1024:(cb + 1) * 1024],
                          odd_w_in[:, cb * 1024:(cb + 1) * 1024].rearrange("(j p) n -> p j n", p=128), [], [w1.r])
                wsf = A([128, 4, 128], F32, "wsf")
                wsT = A([128, 4, 128], BF16, "wsT")
                k.dma("sp", wsf[:], odd_w_s.rearrange("g i j -> i g j"), [], [wsf.r])
                for g in range(4):
                    pb = nps()
                    k.op("pe", lambda e, g=g, pb=pb: e.transpose(out=pb[:, 0:128], in_=wsf[:, g, :], identity=identf[:]),
                         reads=[wsf.r, identf.r], writes=[pb.r])
                    k.op("act", lambda e, g=g, pb=pb: e.copy(out=wsT[:, g, :], in_=pb[:, 0:128]), reads=[pb.r], writes=[wsT.r])
                vnb = A([128, D], F32, "vnb")
                bcast(vnb, lambda j: vnT[:, j:j + 1], 8, vnT.r)
                NX = 4
                xt2 = [A([128, D], F32, "xt2") for _ in range(3)]
                xts = [A([128, D], F32, "xt") for _ in range(NX)]
                hTs = [A([128, 8, 128], BF16, "hT") for _ in range(2)]
                us = [A([128, D], F32, "u") for _ in range(3)]
                vs = [A([128, D], F32, "v") for _ in range(2)]
                vnn = [A([128, D], BF16, "vn") for _ in range(2)]
                mixs = [A([128, D], BF16, "mix") for _ in range(2)]
                mixTs = [A([128, 8, 128], BF16, "mixT") for _ in range(2)]
                junk = A([128, D], BF16, "junk")
                ss = [A([128, 1], F32, "ss") for _ in range(3)]
                rs = [A([128, 1], F32, "rs") for _ in range(3)]
                ss2 = [A([128, 1], F32, "ss2") for _ in range(3)]
                rs2 = [A([128, 1], F32, "rs2") for _ in range(3)]
                work = [(ch, s) for ch in range(16) for s in range(NS)]

                pl_tr = bank_pool([0, 1])
                pl_pj = bank_pool([2, 3])
                pl_sm = bank_pool([4])
                pl_mt = bank_pool([5])

                def s_load(i):
                    ch, s = work[i]
                    k.dma("sp", xts[i % NX][:], xd[s][ch * 128:(ch + 1) * 128, :], [], [xts[i % NX].r])

                def s_sq(i):
                    xt = xts[i % NX]
                    k.op("act", lambda e: e.activation(out=junk[:], in_=xt[:], func=AF.Square, accum_out=ss[i % 3][:, 0:1]),
                         reads=[xt.r], writes=[junk.r, ss[i % 3].r])

                def s_rstd(i):
                    rstd_from_ss(ss[i % 3], rs[i % 3], 1, 1.0 / D)

                def s_norm(i):
                    ch, s = work[i]
                    xt, hT = xts[i % NX], hTs[i % 2]
                    xn = xt
                    k.op("act", lambda e: e.activation(out=xn[:], in_=xt[:], func=AF.Copy, scale=rs[i % 3][:, 0:1]),
                         reads=[xt.r, rs[i % 3].r], writes=[xn.r])
                    for q in range(2):
                        pb = pl_tr()

                        def tr(e, q=q, pb=pb):
                            for c in range(4):
                                kc = q * 4 + c
                                ii = e.transpose(out=pb[:, c * 128:(c + 1) * 128], in_=xn[:, kc * 128:(kc + 1) * 128],
                                                 identity=identf[:])
                            return ii
                        k.op("pe", tr, reads=[xn.r, identf.r], writes=[pb.r])
                        for c in range(4):
                            kc = q * 4 + c
                            k.op("dve", lambda e, kc=kc, c=c, pb=pb: e.tensor_scalar(
                                out=hT[:, kc, :], in0=pb[:, c * 128:(c + 1) * 128], scalar1=a1T[1][:, kc, s:s + 1],
                                scalar2=modT[1][:, kc, s:s + 1], op0=ALU.mult, op1=ALU.add),
                                reads=[pb.r, a1T[1].r, modT[1].r], writes=[hT.r])

                def s_proj(i):
                    hT, u_, v_ = hTs[i % 2], us[i % 3], vs[i % 2]
                    for cb in range(4):
                        pb = pl_pj()

                        def mm(e, cb=cb, pb=pb):
                            for kc in range(8):
                                ii = e.matmul(pb[:], lhsT=hT[:, kc, :], rhs=w1[:, kc, cb * 512:(cb + 1) * 512],
                                              start=(kc == 0), stop=(kc == 7))
                            return ii
                        k.op("pe", mm, reads=[hT.r, w1.r], writes=[pb.r])
                        dst = u_ if cb < 2 else v_
                        k.op("act", lambda e, cb=cb, pb=pb, dst=dst: e.activation(
                            out=dst[:, (cb % 2) * 512:(cb % 2 + 1) * 512], in_=pb[:], func=AF.Gelu),
                            reads=[pb.r], writes=[dst.r])
                    k.op("act", lambda e: e.activation(out=junk[:], in_=v_[:], func=AF.Square, accum_out=ss2[i % 3][:, 0:1]),
                         reads=[v_.r], writes=[junk.r, ss2[i % 3].r])

                def s_rstd2(i):
                    rstd_from_ss(ss2[i % 3], rs2[i % 3], 1, 1.0 / D)

                def s_gate(i):
                    u_, v_, vn_, mix = us[i % 3], vs[i % 2], vnn[i % 2], mixs[i % 2]
                    k.op("dve", lambda e: e.scalar_tensor_tensor(out=vn_[:], in0=v_[:], scalar=rs2[i % 3][:, 0:1], in1=vnb[:],
                                                                 op0=ALU.mult, op1=ALU.mult),
                         reads=[v_.r, rs2[i % 3].r, vnb.r], writes=[vn_.r])
                    for g2_ in range(2):
                        pb = pl_sm()

                        def mms(e, g2_=g2_, pb=pb):
                            for gg in range(2):
                                g = g2_ * 2 + gg
                                ii = e.matmul(pb[:, gg * 256:(gg + 1) * 256], lhsT=wsT[:, g, :],
                                              rhs=vn_[:, g * 256:(g + 1) * 256], start=True, stop=True)
                            return ii
                        k.op("pe", mms, reads=[wsT.r, vn_.r], writes=[pb.r])
                        for gg in range(2):
                            g = g2_ * 2 + gg
                            k.op("dve", lambda e, g=g, gg=gg, pb=pb: e.scalar_tensor_tensor(
                                out=mix[:, g * 256:(g + 1) * 256], in0=pb[:, gg * 256:(gg + 1) * 256],
                                scalar=bsT[:, g:g + 1], in1=u_[:, g * 256:(g + 1) * 256], op0=ALU.add, op1=ALU.mult),
                                reads=[pb.r, bsT.r, u_.r], writes=[mix.r])

                def s_mixT(i):
                    mix, mixT = mixs[i % 2], mixTs[i % 2]
                    pb = pl_mt()
                    pbb = pb[:].bitcast(BF16)

                    def trm(e, pbb=pbb):
                        for kc in range(8):
                            ii = e.transpose(out=pbb[:, kc * 128:(kc + 1) * 128], in_=mix[:, kc * 128:(kc + 1) * 128],
                                             identity=identb[:])
                        return ii
                    k.op("pe", trm, reads=[mix.r, identb.r], writes=[pb.r])
                    k.op("act", lambda e, pbb=pbb: e.copy(out=mixT[:], in_=pbb.rearrange("p (j t) -> p j t", j=8)),
                         reads=[pb.r], writes=[mixT.r])

                def s_load2(i):
                    ch, s = work[i]
                    k.dma("sp", xt2[i % 3][:], xd[s][ch * 128:(ch + 1) * 128, :], [], [xt2[i % 3].r])

                stages = tail_stages(tb, 1, work,
                                     lambda i: ((lambda kc, mT=mixTs[i % 2]: mT[:, kc, :]), mixTs[i % 2].r),
                                     lambda i: (xt2[i % 3][:], xt2[i % 3].r),
                                     [bank_pool([6, 7]), pl_tr, pl_sm, pl_sm])
                run_skewed(len(work), [s_load, s_sq, s_rstd, s_norm, s_proj, s_rstd2, s_gate, s_load2, s_mixT] + stages)
                k.barrier()

        def phase_final(src):
            with contextlib.ExitStack() as st:
                A = lambda shape, dt, name=None: alloc(st, shape, dt, name)
                fnb = A([128, D], F32, "fnb")
                bcast(fnb, lambda j: fnT[:, j:j + 1], 8, fnT.r)
                xts = [A([128, 4, D], F32, "xt") for _ in range(2)]
                ots = [A([128, 4, D], F32, "ot") for _ in range(2)]
                junk = A([128, D], BF16, "junk")
                ss = [A([128, 4], F32, "ss") for _ in range(2)]
                rs = [A([128, 4], F32, "rs") for _ in range(2)]
                work = [(s, tb) for s in range(NS) for tb in range(4)]

                def load(i):
                    s, tb = work[i]
                    k.dma("sp", xts[i % 2][:], src[s][tb * 512:(tb + 1) * 512, :].rearrange("(c p) d -> p c d", p=128),
                          [], [xts[i % 2].r])
                load(0)
                for i, (s, tb) in enumerate(work):
                    if i + 1 < len(work):
                        load(i + 1)
                    xt, ot = xts[i % 2], ots[i % 2]
                    for c in range(4):
                        k.op("act", lambda e, c=c: e.activation(out=junk[:], in_=xt[:, c, :], func=AF.Square,
                                                                accum_out=ss[i % 2][:, c:c + 1]),
                             reads=[xt.r], writes=[junk.r, ss[i % 2].r])
                    rstd_from_ss(ss[i % 2], rs[i % 2], 4, 1.0 / D)
                    for c in range(4):
                        k.op("dve", lambda e, c=c: e.scalar_tensor_tensor(
                            out=ot[:, c, :], in0=xt[:, c, :], scalar=rs[i % 2][:, c:c + 1], in1=fnb[:], op0=ALU.mult,
                            op1=ALU.mult), reads=[xt.r, rs[i % 2].r, fnb.r], writes=[ot.r])
                    k.dma("sp", out_d[s][tb * 512:(tb + 1) * 512, :].rearrange("(c p) d -> p c d", p=128), ot[:],
                          [ot.r], [dummy])
                k.barrier()

        stages = ["l0_inproj", "l0_attn", "l0_tail", "moe0", "l1", "moe1"]
        fns = {"l0_inproj": phase_l0_inproj, "l0_conv": phase_l0_conv, "l0_attn": phase_l0_attn,
               "l0_tail": phase_l0_tail, "topk0": phase_topk, "moe0": lambda: phase_moe(0), "l1": phase_l1,
               "topk1": phase_topk, "moe1": lambda: phase_moe(1)}
        k.barrier()
        for sname in stages:
            fns[sname]()
            if stop == sname:
                break
        if stop in ("all", "moe1"):
            phase_final(xd)
        elif stop in ("l0_tail", "topk0", "moe0", "l1", "topk1"):
            k.dma("sp", dbg_idx, idxT[:], [idxT.r], [dummy])
            k.dma("sp", dbg_val, valT[:], [valT.r], [dummy])
            with contextlib.ExitStack() as st:
                xt = alloc(st, [128, 4, D], F32, "dbg")
                for s in range(NS):
                    for tb in range(4):
                        k.dma("sp", xt[:], xd[s][tb * 512:(tb + 1) * 512, :].rearrange("(c p) d -> p c d", p=128), [], [xt.r])
                        k.dma("sp", out_d[s][tb * 512:(tb + 1) * 512, :].rearrange("(c p) d -> p c d", p=128), xt[:],
                              [xt.r], [dummy])
                k.barrier()
        k.finish()
    print("built: %d instructions, %d waits" % (k.n_inst, k.n_wait))
    return nc


def _rope_tables():
    rows = SEQ // 64
    row = np.repeat(np.arange(rows), 64).astype(np.float32)
    col = np.tile(np.arange(64), rows).astype(np.float32)
    half = 32
    inv = (1.0 / (10000.0 ** (np.arange(0, half, 2, dtype=np.float32) / half))).astype(np.float32)
    ang_r = row[:, None] * inv
    ang_c = col[:, None] * inv
    ang = np.concatenate([ang_r, ang_r, ang_c, ang_c], axis=-1)
    cosT = np.ascontiguousarray(np.tile(np.cos(ang).T, (2, 1))).astype(np.float32)
    sinT = np.ascontiguousarray(np.tile(np.sin(ang).T, (2, 1))).astype(np.float32)
    return cosT, sinT


_NC_CACHE = {}


def kernel(x, c, ctx, c_ctx, w_mod, b_mod, norm1, norm2, even_w_in, even_lambda, even_subln, even_conv_w,
           odd_w_in, odd_v_norm, odd_w_s, odd_b_s, w_out, w_router, w_gate, w_up, w_down, final_norm, _stop="all"):
    f = lambda a: np.ascontiguousarray(np.asarray(a, dtype=np.float32))
    if _stop not in _NC_CACHE:
        _NC_CACHE[_stop] = build_nc(_stop)
    nc = _NC_CACHE[_stop]
    cosT, sinT = _rope_tables()
    x, c, ctx, c_ctx = f(x), f(c), f(ctx), f(c_ctx)
    shared = {
        "w_mod": f(w_mod), "b_mod": f(b_mod), "norm1": f(norm1), "norm2": f(norm2),
        "even_w_in": f(even_w_in)[0], "even_lambda": f(even_lambda)[0], "even_subln": f(even_subln)[0],
        "even_conv_w": f(even_conv_w)[0], "odd_w_in": f(odd_w_in)[0], "odd_v_norm": f(odd_v_norm)[0],
        "odd_w_s": f(odd_w_s)[0], "odd_b_s": f(odd_b_s)[0], "w_out": f(w_out), "w_router": f(w_router),
        "w_gate": f(w_gate), "w_up": f(w_up), "w_down": f(w_down), "final_norm": f(final_norm),
        "rope_cos": cosT, "rope_sin": sinT,
    }
    in_maps = []
    for i in range(N_CORES):
        sl = slice(i * NS, (i + 1) * NS)
        m = dict(shared)
        m["x"] = np.ascontiguousarray(x[sl])
        m["ctx"] = np.ascontiguousarray(ctx[sl])
        m["cvec"] = np.ascontiguousarray(np.stack([c[i * NS], c[i * NS + 1], c_ctx, c_ctx], axis=0))
        in_maps.append(m)
    res = run_bass_kernel_spmd(nc, in_maps, core_ids=list(range(N_CORES)))
    if _stop != "all":
        global _DBG
        _DBG = res.results
    return np.concatenate([r["out"] for r in res.results], axis=0).astype(np.float32)
```
